# Optimizing a Trainium2 kernel written in Bass

```python
import jax, jax.numpy as jnp
from jax import lax
import numpy as np

D_MODEL = 1024
BATCH = 4
SEQ = 8192
DEPTH = 2

GRID_W = 64
CTX_LEN = 256
EPS = 1e-6
LN_EPS = 64e-5
MIX_W = D_MODEL
CHUNK = 128
A_GROUPS = 4
A_WIDTH = MIX_W // 2
A_GDIM = A_WIDTH // A_GROUPS
B_HEAD = 64
B_WIDTH = MIX_W // 2
B_HEADS = B_WIDTH // B_HEAD
DECAY_LORA = 64
AAA_LORA = 64
GATE_LORA = 128
C_WIDTH = MIX_W
C_HEADS = 4
C_HEAD = C_WIDTH // C_HEADS
QKV_BLOCK = 4
C_CHUNK = 128
D_FF = 2816
N_EVEN = (DEPTH + 1) // 2
N_ODD = DEPTH // 2
AB_IN = 2 * A_WIDTH + 3 * B_WIDTH + 2 * DECAY_LORA + 2 * AAA_LORA + GATE_LORA
C_IN = 2 * C_WIDTH + 4 * C_HEADS

kernel_name = 'hybrid_gmlp_rwkv7_mlstm_dit_block'


def rmsnorm(x, g):
    xf = x.astype(jnp.float32)
    y = xf * lax.rsqrt(jnp.mean(xf * xf, axis=-1, keepdims=True) + EPS)
    return (y * g).astype(x.dtype)


def modulate(h, shift, scale):
    return h * (1 + scale) + shift


def ada_mod(cvec, w, b):
    m = jax.nn.silu(cvec) @ w + b
    m = m.reshape(cvec.shape[:-1] + (6, D_MODEL))
    if cvec.ndim == 2:
        return [m[:, i, None, :] for i in range(6)]
    return [m[i] for i in range(6)]


def dwconv1d(x, w):
    xp = jnp.pad(x, ((0, 0), (1, 1), (0, 0)))
    return xp[:, :-2] * w[0] + xp[:, 1:-1] * w[1] + xp[:, 2:] * w[2]


def dwconv_grid(x, w, rows):
    bn, t, ch = x.shape
    y = lax.conv_general_dilated(x.reshape(bn, rows, GRID_W, ch), w[:, :, None, :],
                                 window_strides=(1, 1), padding='SAME',
                                 dimension_numbers=('NHWC', 'HWIO', 'NHWC'),
                                 feature_group_count=ch)
    return y.reshape(bn, t, ch)


def conv_ffn(h, w_up, conv_w, conv_b, w_down, rows):
    a, b = jnp.split(h @ w_up, 2, axis=-1)
    a = dwconv1d(a, conv_w[1]) if rows is None else dwconv_grid(a, conv_w, rows)
    return (jax.nn.gelu(a + conv_b) * b) @ w_down


def spatial_gate(p, w_s, b_s, g_v):
    bn, t, _ = p.shape
    u, v = jnp.split(jax.nn.gelu(p[..., :2 * A_WIDTH]), 2, axis=-1)
    v = rmsnorm(v.reshape(bn, t // CHUNK, CHUNK, A_GROUPS, A_GDIM), g_v)
    z = jnp.einsum('gpq,bnqgc->bnpgc', w_s, v) + b_s.T[None, None, :, :, None]
    return u * z.reshape(bn, t, A_WIDTH)


def rwkv_prep(p, b_shift, b_w0, b_w_up, b_a0, b_a_up, b_g_up, b_kk, b_ka):
    bn, t, _ = p.shape
    o = 2 * A_WIDTH
    r, k, v = jnp.split(dwconv1d(p[..., o:o + 3 * B_WIDTH], b_shift), 3, axis=-1)
    o += 3 * B_WIDTH
    wd = p[..., o:o + 2 * DECAY_LORA].reshape(bn, t, 2, DECAY_LORA)
    o += 2 * DECAY_LORA
    ad = p[..., o:o + 2 * AAA_LORA].reshape(bn, t, 2, AAA_LORA)
    o += 2 * AAA_LORA
    gd = p[..., o:o + GATE_LORA]
    logw = -jax.nn.softplus(-(b_w0 + jnp.einsum('btdr,drc->btdc', jnp.tanh(wd), b_w_up))) - 0.5
    decay = jnp.exp(-jnp.exp(logw.astype(jnp.float32)))
    a = jax.nn.sigmoid(b_a0 + jnp.einsum('btdr,drc->btdc', ad, b_a_up))
    g = jax.nn.sigmoid(gd) @ b_g_up
    hs = lambda u: u.reshape(u.shape[:-1] + (B_HEADS, B_HEAD))
    kk = hs(k * b_kk).astype(jnp.float32)
    kk = (kk * lax.rsqrt(jnp.maximum(jnp.sum(kk * kk, axis=-1, keepdims=True), 1e-12))).astype(k.dtype)
    k_dir = k[:, :, None, :] * (1 + (a - 1) * b_ka)
    return hs(r), hs(decay), hs(k_dir), hs(v), kk, hs(a), g


def rwkv_dir_inputs(prep, d):
    r, w, kd, v, kk, a, _ = prep
    seq = (r, w[:, :, d], kd[:, :, d], v, kk, kk * a[:, :, d])
    return tuple(u[:, ::-1] for u in seq) if d == 1 else seq


def rwkv7_scan(r, w, k, v, kk, b, s0):
    dt = r.dtype
    def step(s, inp):
        r_t, w_t, k_t, v_t, kk_t, b_t = inp
        sa = jnp.einsum('bhvk,bhk->bhv', s, kk_t)
        s = s * w_t[:, :, None, :] - sa[..., None] * b_t[:, :, None, :] + v_t[..., None] * k_t[:, :, None, :]
        return s, jnp.einsum('bhvk,bhk->bhv', s, r_t)
    xs = tuple(jnp.moveaxis(u.astype(jnp.float32), 1, 0) for u in (r, w, k, v, kk, b))
    s, ys = lax.scan(step, s0, xs)
    return jnp.moveaxis(ys, 0, 1).astype(dt), s


def rwkv_out(y, prep, b_rk, ln_g, ln_b):
    r, _, kd, v, _, _, g = prep
    yf = y.astype(jnp.float32)
    mu = jnp.mean(yf, axis=-1, keepdims=True)
    var = jnp.mean(jnp.square(yf - mu), axis=-1, keepdims=True)
    yn = ((yf - mu) * lax.rsqrt(var + LN_EPS)).astype(r.dtype) * ln_g + ln_b
    bonus = jnp.sum(jnp.sum(r[:, :, None] * kd * b_rk, axis=-1, keepdims=True), axis=2) * v
    out = (yn + bonus) * g.reshape(v.shape)
    return out.reshape(v.shape[0], v.shape[1], B_WIDTH)


def rwkv_bidir(pc, pl, need_ctx, b_shift, b_w0, b_w_up, b_a0, b_a_up, b_g_up, b_kk, b_ka, b_rk, b_ln_g, b_ln_b):
    prep_c = rwkv_prep(pc, b_shift, b_w0, b_w_up, b_a0, b_a_up, b_g_up, b_kk, b_ka)
    prep_l = rwkv_prep(pl, b_shift, b_w0, b_w_up, b_a0, b_a_up, b_g_up, b_kk, b_ka)
    bn = pl.shape[0]
    ys_c, ys_l = [], []
    for d in range(2):
        s0 = jnp.zeros((bn, B_HEADS, B_HEAD, B_HEAD), jnp.float32)
        y_c, s_c = rwkv7_scan(*rwkv_dir_inputs(prep_c, d), s0)
        y_l, _ = rwkv7_scan(*rwkv_dir_inputs(prep_l, d), s_c)
        if d == 1:
            y_c, y_l = y_c[:, ::-1], y_l[:, ::-1]
        ys_c.append(y_c)
        ys_l.append(y_l)
    out_l = rwkv_out(ys_l[0] + ys_l[1], prep_l, b_rk, b_ln_g, b_ln_b)
    out_c = rwkv_out(ys_c[0] + ys_c[1], prep_c, b_rk, b_ln_g, b_ln_b) if need_ctx else None
    return out_c, out_l


def mixer_ab(hc, hl, need_ctx, w_in, w_out, a_ws, a_bs, a_gv, b_shift, b_w0, b_w_up, b_a0, b_a_up,
             b_g_up, b_kk, b_ka, b_rk, b_ln_g, b_ln_b):
    pc, pl = hc @ w_in, hl @ w_in
    bc, bl = rwkv_bidir(pc, pl, need_ctx, b_shift, b_w0, b_w_up, b_a0, b_a_up, b_g_up, b_kk, b_ka,
                        b_rk, b_ln_g, b_ln_b)
    yl = jnp.concatenate([spatial_gate(pl, a_ws, a_bs, a_gv), bl], axis=-1) @ w_out
    yc = jnp.concatenate([spatial_gate(pc, a_ws, a_bs, a_gv), bc], axis=-1) @ w_out if need_ctx else None
    return yc, yl


def mlstm_prep(p, conv, conv_b, wq, wk, wv, bi, bf):
    bn, t, _ = p.shape
    xm, z = p[..., :C_WIDTH], p[..., C_WIDTH:2 * C_WIDTH]
    gates = p[..., 2 * C_WIDTH:].reshape(bn, t, 2, 2, C_HEADS)
    xcv = jax.nn.silu(dwconv1d(xm, conv) + conv_b)
    def blockdiag(u, w):
        return jnp.einsum('btgi,gio->btgo', u.reshape(bn, t, C_WIDTH // QKV_BLOCK, QKV_BLOCK), w).reshape(bn, t, C_WIDTH)
    def heads(u):
        return u.reshape(bn, t, C_HEADS, C_HEAD).transpose(0, 2, 1, 3)
    q = heads(blockdiag(xcv, wq))
    k = heads(blockdiag(xcv, wk)) * (C_HEAD ** -0.5)
    v = heads(blockdiag(xm, wv))
    logi = jnp.transpose(gates[:, :, 0] + bi, (2, 0, 3, 1))
    logf = jax.nn.log_sigmoid(jnp.transpose(gates[:, :, 1] + bf, (2, 0, 3, 1)))
    return q, k, v, logi, logf, xcv, z


def mlstm_dir_inputs(prep, d):
    q, k, v, logi, logf = prep[:5]
    seq = (q, k, v, logi[d], logf[d])
    return tuple(u[:, :, ::-1] for u in seq) if d == 1 else seq


def mlstm_chunk_scan(q, k, v, logi, logf, state):
    bn, nh, t, dh = q.shape
    nc = t // C_CHUNK
    dt = q.dtype
    tril = jnp.tril(jnp.ones((C_CHUNK, C_CHUNK), dtype=bool))
    def chunks(u):
        u = u.astype(jnp.float32)
        return jnp.moveaxis(u.reshape((bn, nh, nc, C_CHUNK) + u.shape[3:]), 2, 0)
    def step(carry, inp):
        cmat, nvec, m = carry
        qc, kc, vc, li, lf = inp
        b = jnp.cumsum(lf, axis=-1)
        logd = jnp.where(tril, b[..., :, None] - b[..., None, :] + li[..., None, :], -jnp.inf)
        inter = b + m[..., None]
        m_row = jnp.maximum(inter, jnp.max(logd, axis=-1))
        s = jnp.einsum('bhjd,bhsd->bhjs', qc, kc) * jnp.exp(logd - m_row[..., None])
        w_inter = jnp.exp(inter - m_row)
        num = jnp.einsum('bhjs,bhsv->bhjv', s, vc) + w_inter[..., None] * jnp.einsum('bhvd,bhjd->bhjv', cmat, qc)
        den = jnp.sum(s, axis=-1) + w_inter * jnp.einsum('bhd,bhjd->bhj', nvec, qc)
        h = num / jnp.maximum(jnp.abs(den), jnp.exp(-m_row))[..., None]
        b_last = b[..., -1]
        log_in = b_last[..., None] - b + li
        m_new = jnp.maximum(b_last + m, jnp.max(log_in, axis=-1))
        carry_scale = jnp.exp(b_last + m - m_new)
        kw = kc * jnp.exp(log_in - m_new[..., None])[..., None]
        cmat = carry_scale[..., None, None] * cmat + jnp.einsum('bhsv,bhsd->bhvd', vc, kw)
        nvec = carry_scale[..., None] * nvec + jnp.sum(kw, axis=-2)
        return (cmat, nvec, m_new), h
    state, hs = lax.scan(step, state, tuple(chunks(u) for u in (q, k, v, logi, logf)))
    h = jnp.moveaxis(hs, 0, 2).reshape(bn, nh, t, v.shape[-1])
    return h.astype(dt), state


def mlstm_out(h, xcv, z, c_norm, c_skip, w_out):
    bn, nh, t, dh = h.shape
    hn = rmsnorm(h.transpose(0, 2, 1, 3), c_norm).reshape(bn, t, C_WIDTH)
    return ((hn + c_skip * xcv) * jax.nn.sigmoid(z)) @ w_out


def mixer_mlstm(hc, hl, need_ctx, w_in, w_out, conv, conv_b, wq, wk, wv, bi, bf, c_norm, c_skip):
    prep_c = mlstm_prep(hc @ w_in, conv, conv_b, wq, wk, wv, bi, bf)
    prep_l = mlstm_prep(hl @ w_in, conv, conv_b, wq, wk, wv, bi, bf)
    bn = hl.shape[0]
    hs_c, hs_l = [], []
    for d in range(2):
        init = (jnp.zeros((bn, C_HEADS, C_HEAD, C_HEAD), jnp.float32),
                jnp.zeros((bn, C_HEADS, C_HEAD), jnp.float32),
                jnp.zeros((bn, C_HEADS), jnp.float32))
        h_c, st = mlstm_chunk_scan(*mlstm_dir_inputs(prep_c, d), init)
        h_l, _ = mlstm_chunk_scan(*mlstm_dir_inputs(prep_l, d), st)
        if d == 1:
            h_c, h_l = h_c[:, :, ::-1], h_l[:, :, ::-1]
        hs_c.append(h_c)
        hs_l.append(h_l)
    yl = mlstm_out(hs_l[0] + hs_l[1], prep_l[5], prep_l[6], c_norm, c_skip, w_out)
    yc = mlstm_out(hs_c[0] + hs_c[1], prep_c[5], prep_c[6], c_norm, c_skip, w_out) if need_ctx else None
    return yc, yl


def setup_inputs(seed: int = 0) -> dict:
    key = jax.random.key(seed)
    ks = iter(jax.random.split(key, 64))
    def nrm(shape, s):
        return jax.random.normal(next(ks), shape, jnp.float32) * s
    D = D_MODEL
    return {
        'x': nrm((BATCH, SEQ, D), 1.0),
        'c': nrm((BATCH, D), 1.0),
        'ctx': nrm((BATCH, CTX_LEN, D), 1.0),
        'c_ctx': nrm((D,), 1.0),
        'ada_w': nrm((DEPTH, D, 6 * D), D ** -0.5),
        'ada_b': nrm((DEPTH, 6 * D), 0.02),
        'norm_mix_pre': 1.0 + nrm((DEPTH, D), 0.05),
        'norm_mix_post': 1.0 + nrm((DEPTH, D), 0.05),
        'norm_ffn_pre': 1.0 + nrm((DEPTH, D), 0.05),
        'norm_ffn_post': 1.0 + nrm((DEPTH, D), 0.05),
        'ffn_up': nrm((DEPTH, D, 2 * D_FF), D ** -0.5),
        'ffn_conv': nrm((DEPTH, 3, 3, D_FF), 1.0 / 3.0),
        'ffn_conv_b': nrm((DEPTH, D_FF), 0.02),
        'ffn_down': nrm((DEPTH, D_FF, D), D_FF ** -0.5),
        'ab_w_in': nrm((N_EVEN, D, AB_IN), D ** -0.5),
        'ab_w_out': nrm((N_EVEN, MIX_W, D), MIX_W ** -0.5),
        'a_ws': nrm((N_EVEN, A_GROUPS, CHUNK, CHUNK), CHUNK ** -0.5),
        'a_bs': 1.0 + nrm((N_EVEN, A_GROUPS, CHUNK), 0.1),
        'a_gv': 1.0 + nrm((N_EVEN, A_GROUPS, A_GDIM), 0.05),
        'b_shift': nrm((N_EVEN, 3, 3 * B_WIDTH), 0.5),
        'b_w0': jnp.linspace(-6.0, -1.0, B_WIDTH, dtype=jnp.float32) + nrm((N_EVEN, 2, B_WIDTH), 0.1),
        'b_w_up': nrm((N_EVEN, 2, DECAY_LORA, B_WIDTH), 0.5 * DECAY_LORA ** -0.5),
        'b_a0': nrm((N_EVEN, 2, B_WIDTH), 0.1),
        'b_a_up': nrm((N_EVEN, 2, AAA_LORA, B_WIDTH), AAA_LORA ** -0.5),
        'b_g_up': nrm((N_EVEN, GATE_LORA, B_WIDTH), GATE_LORA ** -0.5),
        'b_kk': 0.85 + nrm((N_EVEN, B_WIDTH), 0.05),
        'b_ka': 1.0 + nrm((N_EVEN, B_WIDTH), 0.05),
        'b_rk': nrm((N_EVEN, B_HEADS, B_HEAD), 0.1),
        'b_ln_g': 1.0 + nrm((N_EVEN, B_HEADS, B_HEAD), 0.05),
        'b_ln_b': nrm((N_EVEN, B_HEADS, B_HEAD), 0.02),
        'c_w_in': nrm((N_ODD, D, C_IN), D ** -0.5),
        'c_w_out': nrm((N_ODD, C_WIDTH, D), C_WIDTH ** -0.5),
        'c_conv': nrm((N_ODD, 3, C_WIDTH), 0.5),
        'c_conv_b': nrm((N_ODD, C_WIDTH), 0.02),
        'c_wq': nrm((N_ODD, C_WIDTH // QKV_BLOCK, QKV_BLOCK, QKV_BLOCK), QKV_BLOCK ** -0.5),
        'c_wk': nrm((N_ODD, C_WIDTH // QKV_BLOCK, QKV_BLOCK, QKV_BLOCK), QKV_BLOCK ** -0.5),
        'c_wv': nrm((N_ODD, C_WIDTH // QKV_BLOCK, QKV_BLOCK, QKV_BLOCK), QKV_BLOCK ** -0.5),
        'c_bi': nrm((N_ODD, 2, C_HEADS), 0.1),
        'c_bf': jnp.linspace(3.0, 6.0, C_HEADS, dtype=jnp.float32) + nrm((N_ODD, 2, C_HEADS), 0.1),
        'c_norm': 1.0 + nrm((N_ODD, C_HEADS, C_HEAD), 0.05),
        'c_skip': 1.0 + nrm((N_ODD, C_WIDTH), 0.05),
    }


def reference(x, c, ctx, c_ctx, ada_w, ada_b, norm_mix_pre, norm_mix_post, norm_ffn_pre, norm_ffn_post,
              ffn_up, ffn_conv, ffn_conv_b, ffn_down, ab_w_in, ab_w_out, a_ws, a_bs, a_gv, b_shift, b_w0,
              b_w_up, b_a0, b_a_up, b_g_up, b_kk, b_ka, b_rk, b_ln_g, b_ln_b, c_w_in, c_w_out, c_conv,
              c_conv_b, c_wq, c_wk, c_wv, c_bi, c_bf, c_norm, c_skip):
    rows = x.shape[1] // GRID_W
    xl, xc = x, ctx
    for layer in range(DEPTH):
        need_ctx = layer < DEPTH - 1
        i = layer // 2
        ml = ada_mod(c, ada_w[layer], ada_b[layer])
        mc = ada_mod(c_ctx, ada_w[layer], ada_b[layer])
        hl = modulate(rmsnorm(xl, norm_mix_pre[layer]), ml[0], ml[1])
        hc = modulate(rmsnorm(xc, norm_mix_pre[layer]), mc[0], mc[1])
        if layer % 2 == 0:
            yc, yl = mixer_ab(hc, hl, need_ctx, ab_w_in[i], ab_w_out[i], a_ws[i], a_bs[i], a_gv[i],
                              b_shift[i], b_w0[i], b_w_up[i], b_a0[i], b_a_up[i], b_g_up[i], b_kk[i],
                              b_ka[i], b_rk[i], b_ln_g[i], b_ln_b[i])
        else:
            yc, yl = mixer_mlstm(hc, hl, need_ctx, c_w_in[i], c_w_out[i], c_conv[i], c_conv_b[i],
                                 c_wq[i], c_wk[i], c_wv[i], c_bi[i], c_bf[i], c_norm[i], c_skip[i])
        xl = xl + ml[2] * rmsnorm(yl, norm_mix_post[layer])
        hl = modulate(rmsnorm(xl, norm_ffn_pre[layer]), ml[3], ml[4])
        xl = xl + ml[5] * rmsnorm(conv_ffn(hl, ffn_up[layer], ffn_conv[layer], ffn_conv_b[layer],
                                           ffn_down[layer], rows), norm_ffn_post[layer])
        if need_ctx:
            xc = xc + mc[2] * rmsnorm(yc, norm_mix_post[layer])
            hc = modulate(rmsnorm(xc, norm_ffn_pre[layer]), mc[3], mc[4])
            xc = xc + mc[5] * rmsnorm(conv_ffn(hc, ffn_up[layer], ffn_conv[layer], ffn_conv_b[layer],
                                               ffn_down[layer], None), norm_ffn_post[layer])
    return xl
```

```python
from contextlib import ExitStack
import concourse.bass as bass
import concourse.mybir as mybir

EPOCH = 8192
NDMA = 12


class V:
    __slots__ = ("ap", "keys")

    def __init__(self, ap, keys):
        self.ap = ap
        assert isinstance(keys, tuple) and all(isinstance(k, tuple) for k in keys), keys
        self.keys = keys


class Buf:
    def __init__(self, t, name):
        self.t = t
        self.name = name

    def __getitem__(self, idx):
        if isinstance(idx, tuple) and len(idx) >= 2 and isinstance(idx[1], int) and len(self.t.shape) >= 3:
            return V(self.t[idx], ((self.name, idx[1]),))
        return V(self.t[idx], ((self.name,),))

    def sub(self, sub, idx):
        return V(self.t[idx], ((self.name, sub),))

    def v(self, ap, sub=None):
        return V(ap, (((self.name,),) if sub is None else ((self.name, sub),)))


class Sched:
    def __init__(self, nc, same_engine_sync=True):
        self.nc = nc
        self.es = ExitStack()
        self.engs = {"pe": nc.tensor, "dve": nc.vector, "act": nc.scalar, "pool": nc.gpsimd, "sp": nc.sync}
        self.count = {e: 0 for e in self.engs}
        self.sems = {}
        self.waited = {e: {f: 0 for f in self.engs} for e in self.engs}
        self.dma_sems = {}
        self.dma_count = {e: 0 for e in self.engs}
        self.dma_waited = {e: {} for e in self.engs}
        self.state = {}
        self.same = same_engine_sync
        self.nwaits = 0
        self.stacks = [self.es]
        self.pe_incs = []
        self.pe_last = None

    def push(self):
        self.stacks.append(ExitStack())

    def pop(self):
        self.barrier()
        self.stacks.pop().close()

    def sbuf(self, name, shape, dt):
        self.uid = getattr(self, "uid", 0) + 1
        name = f"{name}_{self.uid}"
        t = self.stacks[-1].enter_context(self.nc.sbuf_tensor(name, list(shape), dt))
        return Buf(t, name)

    def psum(self, name, shape, dt):
        t = self.stacks[-1].enter_context(self.nc.psum_tensor(name, list(shape), dt))
        return Buf(t, name)

    def _sem(self, e, ep):
        k = (e, ep)
        if k not in self.sems:
            self.sems[k] = self.es.enter_context(self.nc.semaphore(f"s_{e}_{ep}"))
        return self.sems[k]

    def _dsem(self, e, i):
        k = (e, i)
        if k not in self.dma_sems:
            self.dma_sems[k] = self.es.enter_context(self.nc.semaphore(f"d_{e}_{i}"))
        return self.dma_sems[k]

    def _entries(self, key):
        name = key[0]
        d = self.state.setdefault(name, {})
        if len(key) == 1:
            if None not in d:
                d[None] = [None, []]
            return list(d.values())
        sub = key[1]
        if sub not in d:
            d[sub] = [None, []]
        out = [d[sub]]
        if None in d:
            out.append(d[None])
        return out

    def _collect(self, reads, writes):
        deps = []
        for v in reads:
            for key in v.keys:
                for ent in self._entries(key):
                    if ent[0] is not None:
                        deps.append(ent[0])
        for v in writes:
            for key in v.keys:
                for ent in self._entries(key):
                    if ent[0] is not None:
                        deps.append(ent[0])
                    deps.extend(ent[1])
        return deps

    def _emit_waits(self, e, deps):
        eng = self.engs[e]
        need = {}
        for d in deps:
            if d[0] == "eng":
                _, f, n = d
                if f == e and (not self.same or e == "pe"):
                    continue
                if self.waited[e][f] >= n:
                    continue
                need[("eng", f)] = max(need.get(("eng", f), 0), n)
            else:
                _, q, i, val = d
                if self.dma_waited[e].get((q, i), 0) >= val:
                    continue
                need[("dma", q, i)] = max(need.get(("dma", q, i), 0), val)
        for k, n in need.items():
            if k[0] == "eng" and k[1] == "pe":
                import bisect
                i = bisect.bisect_left(self.pe_incs, (n, 0))
                if i == len(self.pe_incs):
                    inst, seq = self.pe_last
                    assert seq >= n, (seq, n)
                    idx = len(self.pe_incs) + 1
                    ep = (idx - 1) // EPOCH
                    inst.then_inc(self._sem("pe", ep), 1)
                    self.pe_incs.append((seq, idx))
                    self.pe_last = None
                seq, idx = self.pe_incs[i]
                ep = (idx - 1) // EPOCH
                eng.wait_ge(self._sem("pe", ep), idx - ep * EPOCH)
                self.waited[e]["pe"] = seq
            elif k[0] == "eng":
                f = k[1]
                ep = (n - 1) // EPOCH
                eng.wait_ge(self._sem(f, ep), n - ep * EPOCH)
                self.waited[e][f] = n
            else:
                _, q, i = k
                eng.wait_ge(self._dsem(q, i), n)
                self.dma_waited[e][(q, i)] = n
            self.nwaits += 1

    def _record(self, dep, reads, writes):
        for v in reads:
            for key in v.keys:
                ents = self._entries(key)
                if len(key) == 1:
                    for ent in ents:
                        ent[1].append(dep)
                else:
                    ents[0][1].append(dep)
        for v in writes:
            for key in v.keys:
                name = key[0]
                if len(key) == 1:
                    self.state[name] = {None: [dep, []]}
                else:
                    d = self.state[name]
                    d[key[1]] = [dep, []]
                    if None in d:
                        pass

    def op(self, e, fn, reads=(), writes=()):
        deps = self._collect(reads, writes)
        self._emit_waits(e, deps)
        n = self.count[e] + 1
        ep = (n - 1) // EPOCH
        inst = fn()
        if e == "pe":
            self.pe_last = (inst, n)
        else:
            inst.then_inc(self._sem(e, ep), 1)
        self.count[e] = n
        self._record(("eng", e, n), reads, writes)
        return inst

    def dma(self, q, out, in_, **kw):
        eng = self.engs[q]
        deps = self._collect([in_], [out])
        i = self.dma_count[q] % NDMA
        use = self.dma_count[q] // NDMA
        if use > 0:
            deps.append(("dma", q, i, 16 * use))
        self._emit_waits(q, deps)
        inst = eng.dma_start(out=out.ap, in_=in_.ap, **kw)
        inst.then_inc(self._dsem(q, i), 16)
        self.dma_count[q] += 1
        self._record(("dma", q, i, 16 * (use + 1)), [in_], [out])
        return inst

    def barrier(self):
        deps = [("eng", f, self.count[f]) for f in self.engs if self.count[f] > 0]
        for q in self.engs:
            c = self.dma_count[q]
            for i in range(min(c, NDMA)):
                uses = (c - 1 - i) // NDMA + 1
                deps.append(("dma", q, i, 16 * uses))
        for e in self.engs:
            self._emit_waits(e, deps)
        self.state = {}

    def wait_all(self, e):
        deps = [("eng", f, self.count[f]) for f in self.engs if self.count[f] > 0 and f != e]
        for q in self.engs:
            c = self.dma_count[q]
            for i in range(min(c, NDMA)):
                uses = (c - 1 - i) // NDMA + 1
                deps.append(("dma", q, i, 16 * uses))
        self._emit_waits(e, deps)

    def matmul(self, out, lhsT, rhs, start=True, stop=True, **kw):
        return self.op("pe", lambda: self.nc.tensor.matmul(out.ap, lhsT.ap, rhs.ap, start=start, stop=stop, **kw),
                       reads=[lhsT, rhs] + ([] if start else [out]), writes=[out])

    def transpose(self, out, in_, ident):
        return self.op("pe", lambda: self.nc.tensor.transpose(out.ap, in_.ap, ident.ap), reads=[in_, ident], writes=[out])

    def act(self, out, in_, func, bias=None, scale=None, accum_out=None, eng="act"):
        kw = {}
        reads = [in_]
        writes = [out]
        if bias is not None:
            if isinstance(bias, V):
                kw["bias"] = bias.ap
                reads.append(bias)
            else:
                kw["bias"] = bias
        if scale is not None:
            if isinstance(scale, V):
                kw["scale"] = scale.ap
                reads.append(scale)
            else:
                kw["scale"] = scale
        if accum_out is not None:
            kw["accum_out"] = accum_out.ap
            writes.append(accum_out)
        return self.op("act", lambda: self.nc.scalar.activation(out.ap, in_.ap, func, **kw), reads=reads, writes=writes)

    def tt(self, e, out, in0, in1, op):
        return self.op(e, lambda: self.engs[e].tensor_tensor(out.ap, in0.ap, in1.ap, op), reads=[in0, in1], writes=[out])

    def ts(self, e, out, in0, s1, op0, s2=None, op1=None):
        reads = [in0]
        a1 = s1
        a2 = s2
        if isinstance(s1, V):
            reads.append(s1)
            a1 = s1.ap
        if isinstance(s2, V):
            reads.append(s2)
            a2 = s2.ap
        if op1 is None:
            return self.op(e, lambda: self.engs[e].tensor_scalar(out.ap, in0.ap, a1, None, op0), reads=reads, writes=[out])
        return self.op(e, lambda: self.engs[e].tensor_scalar(out.ap, in0.ap, a1, a2, op0, op1), reads=reads, writes=[out])

    def stt(self, out, in0, s, in1, op0, op1, e="dve"):
        reads = [in0, in1]
        a = s
        if isinstance(s, V):
            reads.append(s)
            a = s.ap
        return self.op(e, lambda: self.engs[e].scalar_tensor_tensor(out.ap, in0.ap, a, in1.ap, op0, op1), reads=reads, writes=[out])

    def copy(self, e, out, in_):
        if e == "act":
            return self.op(e, lambda: self.nc.scalar.copy(out.ap, in_.ap), reads=[in_], writes=[out])
        return self.op(e, lambda: self.engs[e].tensor_copy(out.ap, in_.ap), reads=[in_], writes=[out])

    def memset(self, e, out, val):
        return self.op(e, lambda: self.engs[e].memset(out.ap, val), reads=[], writes=[out])

import numpy as np
import concourse.bass as bass
import concourse.mybir as mybir
from concourse.bass_utils import run_bass_kernel_spmd

F32 = mybir.dt.float32
BF16 = mybir.dt.bfloat16
AF = mybir.ActivationFunctionType
ALU = mybir.AluOpType

D = 1024
KD = 8
DFF = 2816
NF = 22
EPS = 1e-6
LN_EPS = 64e-5
TS = 256
LR = 64
LM = 128


def vec_layout():
    lay = {}
    off = 0

    def add(name, n):
        nonlocal off
        lay[name] = (off, n)
        off += n
    for l in range(2):
        for nm in ("nmp", "nmo", "nfp", "nfo"):
            add(f"{nm}{l}", 8)
        add(f"adab{l}", 48)
        for i in range(9):
            add(f"fconv{l}_{i}", NF)
        add(f"fcb{l}", NF)
    add("agv", 4)
    for i in range(3):
        add(f"bshift{i}", 12)
    add("bw0", 8)
    add("ba0", 8)
    add("bkk", 4)
    add("bka", 4)
    add("brk", 4)
    add("blng", 4)
    add("blnb", 4)
    for i in range(3):
        add(f"cconv{i}", 8)
    add("ccb", 8)
    add("cskip", 8)
    add("cnorm", 8)
    add("cgb", 1)
    return lay, off


def const_layout():
    lay = {}
    off = 0
    for name, n in (("ident", 128), ("ones", 128), ("blk64", 128), ("su", 512), ("sl", 512), ("iu", 512), ("il", 512),
                    ("id8", 512), ("mu", 128), ("ml", 128), ("sel", 2048), ("seg", TS)):
        lay[name] = (off, n)
        off += n
    return lay, off


def make_consts():
    lay, n = const_layout()
    c = np.zeros((128, n), np.float32)
    c[:, lay["ident"][0]:lay["ident"][0] + 128] = np.eye(128)
    c[:, lay["ones"][0]:lay["ones"][0] + 128] = 1.0
    b = np.zeros((128, 128), np.float32)
    b[:64, :64] = 1
    b[64:, 64:] = 1
    c[:, lay["blk64"][0]:lay["blk64"][0] + 128] = b
    su = np.triu(np.ones((64, 64), np.float32), 1)
    sl = np.tril(np.ones((64, 64), np.float32), -1)
    for nm, m in (("su", su), ("sl", sl), ("iu", su + np.eye(64)), ("il", sl + np.eye(64)), ("id8", np.eye(64))):
        c[:64, lay[nm][0]:lay[nm][0] + 512] = np.tile(m, (1, 8))
    c[:, lay["mu"][0]:lay["mu"][0] + 128] = np.triu(np.ones((128, 128), np.float32))
    c[:, lay["ml"][0]:lay["ml"][0] + 128] = np.tril(np.ones((128, 128), np.float32))
    for r in range(16):
        c[r, lay["sel"][0] + r * 128: lay["sel"][0] + (r + 1) * 128] = 1.0
    seg = np.ones((TS,), np.float32)
    seg[::LR] = 0.0
    c[:, lay["seg"][0]:lay["seg"][0] + TS] = seg[None, :]
    return c


class Ctx:
    pass


def build(TL, TC, nlayers=2):

    nc = bass.Bass("TRN2", target_bir_lowering=False)
    S = Sched(nc)
    vlay, NV = vec_layout()
    clay, NCON = const_layout()

    def dram(name, shape, dt=F32, kind="Internal"):
        return Buf(nc.dram_tensor(name, list(shape), dt, kind=kind), name)

    EI = "ExternalInput"
    xT = dram("xT", [D, TL], kind=EI)
    cxT = dram("cxT", [D, TC], kind=EI)
    cvec = dram("cvec", [128, KD, 2], kind=EI)
    vecs_d = dram("vecs", [128, NV], kind=EI)
    consts_d = dram("consts", [128, NCON], kind=EI)
    ada_w = dram("ada_w", [2, D, 6 * D], kind=EI)
    ffn_up = dram("ffn_up", [2, D, 2 * DFF], kind=EI)
    ffn_down = dram("ffn_down", [2, DFF, D], kind=EI)
    ab_w_in = dram("ab_w_in", [D, 2944], kind=EI)
    ab_w_out = dram("ab_w_out", [D, D], kind=EI)
    c_w_in = dram("c_w_in", [D, 2064], kind=EI)
    c_w_out = dram("c_w_out", [D, D], kind=EI)
    a_wsT = dram("a_wsT", [128, 4, 128], kind=EI)
    a_bs_bc = dram("a_bs_bc", [128, 512], kind=EI)
    b_up = dram("b_up", [128, 5, 512], kind=EI)
    c_bd = dram("c_bd", [128, 3, KD, 128], kind=EI)
    outT = dram("outT", [D, TL], kind="ExternalOutput")

    class Stream:
        pass
    lat = Stream(); lat.T = TL; lat.name = "l"; lat.idx = 0
    ctx = Stream(); ctx.T = TC; ctx.name = "c"; ctx.idx = 1
    lat.res = [xT, dram("lR1", [D, TL]), dram("lR2", [D, TL]), dram("lR3", [D, TL]), outT]
    ctx.res = [cxT, dram("cR1", [D, TC]), dram("cR2", [D, TC]), None, None]
    for st in (lat, ctx):
        T = st.T
        n = st.name
        st.AO = dram(n + "AO", [512, T], BF16)
        for nm, rows in (("R", 512), ("KD0", 512), ("KD1", 512), ("VV", 512), ("KK", 512), ("B0", 512), ("B1", 512),
                         ("LD0", 512), ("LD1", 512), ("GG", 512), ("BV", 512), ("Y0", 512), ("Y1", 512),
                         ("FB", DFF),
                         ("MQ", D), ("MK", D), ("XCV", D), ("SZ", D), ("H0", D), ("H1", D)):
            setattr(st, nm, dram(n + nm, [rows, T]))
        st.FA = dram(n + "FA", [DFF, T], BF16)
        st.KTOK = dram(n + "KTOK", [T, D])
        st.VTOK = dram(n + "VTOK", [T, D])
        st.GATE = dram(n + "GATE", [16, T])

    def fm(buf, r0, r1, c0, c1):
        return buf.v(buf.t[r0:r1, c0:c1].rearrange("(j p) t -> p j t", p=128))

    vecs = S.sbuf("vecs_s", [128, NV], F32)
    con = S.sbuf("con_s", [128, NCON], F32)
    mod = S.sbuf("mod_s", [128, 48, 2], F32)
    S.dma("sp", vecs[:], vecs_d[:])
    S.dma("sp", con[:], consts_d[:])

    def vc(name, j=0, n=1):
        o = vlay[name][0] + j
        return vecs[:, o:o + n]

    def vcr(name, rows):
        o = vlay[name][0]
        return vecs[0:rows, o:o + 1]

    def cc(name, rows=128, c0=0, n=None):
        o, w = clay[name]
        if n is None:
            n = w
        return con[0:rows, o + c0:o + c0 + n]

    ones_b = S.sbuf("ones_b", [128, 128], BF16)
    S.memset("dve", ones_b[:], 1.0)
    banks = [S.psum(f"psb{i}", [128, 512], F32) for i in range(8)]
    bank_i = [0]

    def nb():
        b = banks[bank_i[0] % 8]
        bank_i[0] += 1
        return b

    rr = [0]

    def ew():
        rr[0] += 1
        return "dve" if rr[0] % 2 else "pool"

    def ada(l):
        S.push()
        cs = S.sbuf("ada_c", [128, KD, 2], F32)
        sc = S.sbuf("ada_sc", [128, KD, 2], F32)
        S.dma("sp", cs[:], cvec[:])
        S.act(sc[:], cs[:], AF.Silu)
        wt = [S.sbuf(f"ada_w{i}", [128, KD, 768], F32) for i in range(2)]
        for g in range(8):
            w = wt[g % 2]
            S.dma("sp", w[:], ada_w.v(ada_w.t[l, :, g * 768:(g + 1) * 768].rearrange("(j p) f -> p j f", p=128)))
            for jj in range(6):
                col = g * 6 + jj
                ps = nb()
                for k in range(KD):
                    S.matmul(ps[:, 0:2], w[:, k, jj * 128:(jj + 1) * 128], sc[:, k, :], start=(k == 0), stop=(k == KD - 1))
                S.ts("dve", mod[:, col, :], ps[:, 0:2], vc(f"adab{l}", col), ALU.add)
        S.pop()

    def load_w_bf16(dst, src_ap_fn, ncols, stage):
        i = 0
        c0 = 0
        while c0 < ncols:
            c1 = min(c0 + stage[0].t.shape[2], ncols)
            st = stage[i % len(stage)]
            S.dma("sp", st.v(st.t[:, :, 0:c1 - c0]), src_ap_fn(c0, c1))
            S.copy(ew(), dst.v(dst.t[:, :, c0:c1]), st.v(st.t[:, :, 0:c1 - c0]))
            c0 = c1
            i += 1

    def rstd_bcast(sq_chunks, n, out_rstd, ones_v, scale, eps, tmp):
        ps = nb()
        for j, sqv in enumerate(sq_chunks):
            S.matmul(ps[:, 0:n], ones_v, sqv, start=(j == 0), stop=(j == len(sq_chunks) - 1))
        S.ts("dve", tmp, ps[:, 0:n], scale, ALU.mult, eps, ALU.add)
        S.act(tmp, tmp, AF.Sqrt)
        S.op("dve", lambda: nc.vector.reciprocal(out_rstd.ap, tmp.ap), reads=[tmp], writes=[out_rstd])

    def modcol(i, j, st):
        return mod[:, i * 8 + j, st.idx:st.idx + 1]

    def make_coef(l, st, gname, scale_i, out):
        sv = mod[:, scale_i * 8:(scale_i + 1) * 8, st.idx]
        S.tt("dve", out, sv, vc(gname, 0, 8), ALU.mult)
        S.tt("dve", out, out, vc(gname, 0, 8), ALU.add)

    def make_gcoef(l, st, gname, gate_i, out):
        sv = mod[:, gate_i * 8:(gate_i + 1) * 8, st.idx]
        S.tt("dve", out, sv, vc(gname, 0, 8), ALU.mult)

    def norm_mod(x, n, coef, shift_i, st, hout, sq, rstd, tmp):
        for j in range(KD):
            S.act(hout[:, j, 0:n], x[:, j, 0:n], AF.Square)
        rstd_bcast([hout[:, j, 0:n] for j in range(KD)], n, rstd.v(rstd.t[:, 0:n]), ones_b[:], 1.0 / D, EPS, tmp.v(tmp.t[:, 0:n]))
        for j in range(KD):
            S.stt(sq[:, j, 0:n], x[:, j, 0:n], coef.v(coef.t[:, j:j + 1]), rstd.v(rstd.t[:, 0:n]), ALU.mult, ALU.mult)
            S.act(hout[:, j, 0:n], sq[:, j, 0:n], AF.Identity, bias=modcol(shift_i, j, st), scale=1.0)

    def post_norm_res(y, n, gcoef, xin, xout, sq, rstd, tmp, sqb=None):
        if sqb is None:
            S.act(sq.v(sq.t[:, :, 0:n]), y.v(y.t[:, :, 0:n]), AF.Square)
            rstd_bcast([sq.v(sq.t[:, j, 0:n]) for j in range(KD)], n, rstd.v(rstd.t[:, 0:n]), cc("ones"), 1.0 / D, EPS, tmp.v(tmp.t[:, 0:n]))
        else:
            for j in range(KD):
                S.act(sqb[:, j, 0:n], y[:, j, 0:n], AF.Square)
            rstd_bcast([sqb[:, j, 0:n] for j in range(KD)], n, rstd.v(rstd.t[:, 0:n]), ones_b[:], 1.0 / D, EPS, tmp.v(tmp.t[:, 0:n]))
        for j in range(KD):
            S.stt(sq[:, j, 0:n], y[:, j, 0:n], gcoef.v(gcoef.t[:, j:j + 1]), rstd.v(rstd.t[:, 0:n]), ALU.mult, ALU.mult)
            S.tt("dve" if j % 2 else "pool", xout[:, j, 0:n], sq[:, j, 0:n], xin[:, j, 0:n], ALU.add)

    def load_halo(dst, src, t0, n, T, rows0, rows1):
        lo, hi = t0 - 1, t0 + n + 1
        clo, chi = max(lo, 0), min(hi, T)
        if lo < 0:
            S.memset("dve", dst.v(dst.t[:, :, 0:1]), 0.0)
        if hi > T:
            S.memset("dve", dst.v(dst.t[:, :, n + 1:n + 2]), 0.0)
        S.dma("sp", dst.v(dst.t[:, :, clo - lo:chi - lo]), fm(src, rows0, rows1, clo, chi))

    def tiles(T):
        return [(t0, min(TS, T - t0)) for t0 in range(0, T, TS)]

    G = Ctx()
    G.nc, G.S, G.lat, G.ctx = nc, S, lat, ctx
    G.fm, G.vc, G.cc, G.nb, G.ew, G.mod, G.vcr = fm, vc, cc, nb, ew, mod, vcr
    G.ada, G.load_w_bf16, G.rstd_bcast, G.modcol = ada, load_w_bf16, rstd_bcast, modcol
    G.make_coef, G.make_gcoef, G.norm_mod, G.post_norm_res, G.load_halo, G.tiles = make_coef, make_gcoef, norm_mod, post_norm_res, load_halo, tiles
    G.w = dict(ffn_up=ffn_up, ffn_down=ffn_down, ab_w_in=ab_w_in, ab_w_out=ab_w_out, c_w_in=c_w_in, c_w_out=c_w_out,
               a_wsT=a_wsT, a_bs_bc=a_bs_bc, b_up=b_up, c_bd=c_bd)
    G.banks = banks
    return G


def in0_phase(G):
    S, nc, fm, vc, cc, nb, ew = G.S, G.nc, G.fm, G.vc, G.cc, G.nb, G.ew
    W = G.w
    S.push()
    NH = TS + 2
    wb = S.sbuf("i0_wb", [128, KD, 2944], BF16)
    stage = [S.sbuf(f"i0_st{i}", [128, KD, 256], F32) for i in range(2)]
    G.load_w_bf16(wb, lambda c0, c1: W["ab_w_in"].v(W["ab_w_in"].t[:, c0:c1].rearrange("(j p) f -> p j f", p=128)), 2944, stage)
    wsT = S.sbuf("i0_wsT", [128, 4, 128], BF16)
    S.dma("sp", stage[0].v(stage[0].t[:, 0:4, 0:128]), W["a_wsT"][:])
    S.copy("dve", wsT[:], stage[0].v(stage[0].t[:, 0:4, 0:128]))
    bsb = S.sbuf("i0_bsb", [128, 512], F32)
    S.dma("sp", bsb[:], W["a_bs_bc"][:])
    upf = S.sbuf("i0_upf", [128, 5, 512], F32)
    upb = S.sbuf("i0_upb", [128, 5, 512], BF16)
    S.dma("sp", upf[:], W["b_up"][:])
    S.copy("dve", upb[:], upf[:])
    omka = S.sbuf("i0_omka", [128, 4], F32)
    S.ts("dve", omka[:], vc("bka", 0, 4), -1.0, ALU.mult, 1.0, ALU.add)
    coef = S.sbuf("i0_coef", [128, 8], F32)
    xt = S.sbuf("i0_xt", [128, KD, NH], F32)
    sq = S.sbuf("i0_sq", [128, KD, NH], F32)
    rstd = S.sbuf("i0_rstd", [128, NH], F32)
    tmp = S.sbuf("i0_tmp", [128, NH], F32)
    h = S.sbuf("i0_h", [128, KD, NH], BF16)
    ug_2 = [S.sbuf("i0_ug%d" % i, [128, TS], F32) for i in range(2)]
    vg_2 = [S.sbuf("i0_vg%d" % i, [128, TS], F32) for i in range(2)]
    vsq_2 = [S.sbuf("i0_vsq%d" % i, [128, TS], F32) for i in range(2)]
    vr_2 = [S.sbuf("i0_vr%d" % i, [128, TS], F32) for i in range(2)]
    vt2_2 = [S.sbuf("i0_vt2%d" % i, [128, TS], F32) for i in range(2)]
    vtok_2 = [S.sbuf("i0_vtok%d" % i, [128, 128], BF16) for i in range(2)]
    zt_2 = [S.sbuf("i0_zt%d" % i, [128, 128], F32) for i in range(2)]
    ao = S.sbuf("i0_ao", [128, 4, TS], BF16)
    rkv = S.sbuf("i0_rkv", [128, 12, TS], F32)
    ct = S.sbuf("i0_ct", [128, TS], F32)
    lor = S.sbuf("i0_lor", [128, 3, TS], BF16)
    LDt = S.sbuf("i0_LD", [128, 8, TS], F32)
    At = S.sbuf("i0_A", [128, 8, TS], F32)
    Gt = S.sbuf("i0_G", [128, 4, TS], F32)
    kk = S.sbuf("i0_kk", [128, 4, TS], F32)
    kd = S.sbuf("i0_kd", [128, 2, 4, TS], F32)
    bb = S.sbuf("i0_bb", [128, 2, 4, TS], F32)
    rk = S.sbuf("i0_rk", [128, 2, 4, TS], F32)
    bv = S.sbuf("i0_bv", [128, 4, TS], F32)

    for st in (G.ctx, G.lat):
        T = st.T
        G.make_coef(0, st, "nmp0", 1, coef[:])
        for (t0, n) in G.tiles(T):
            nh = n + 2
            G.load_halo(xt, st.res[0], t0, n, T, 0, D)
            G.norm_mod(xt, nh, coef, 0, st, h, sq, rstd, tmp)

            def proj(oc, msz=128):
                ps = nb()
                for k in range(KD):
                    S.matmul(ps[0:msz, 0:nh], wb[:, k, oc * 128:oc * 128 + msz], h[:, k, 0:nh], start=(k == 0), stop=(k == KD - 1))
                return ps
            for g in range(4):
                ug, vg, vsq, vr, vt2 = ug_2[g % 2], vg_2[g % 2], vsq_2[g % 2], vr_2[g % 2], vt2_2[g % 2]
                psu = proj(g)
                S.act(ug[:, 0:n], psu[:, 1:n + 1], AF.Gelu_apprx_tanh)
                psv = proj(4 + g)
                S.act(vg[:, 0:n], psv[:, 1:n + 1], AF.Gelu_apprx_tanh)
                S.act(vsq[:, 0:n], vg[:, 0:n], AF.Square)
                G.rstd_bcast([vsq[:, 0:n]], n, vr[:, 0:n], cc("ones"), 1.0 / 128, EPS, vt2[:, 0:n])
                S.stt(vsq[:, 0:n], vg[:, 0:n], vc("agv", g), vr[:, 0:n], ALU.mult, ALU.mult)
                for c0 in range(0, n, 128):
                    vtok, zt = vtok_2[(c0 // 128) % 2], zt_2[(c0 // 128) % 2]
                    pt = nb()
                    S.transpose(pt[:, 0:128], vsq[:, c0:c0 + 128], cc("ident"))
                    S.copy("act", vtok[:], pt[:, 0:128])
                    pz = nb()
                    S.matmul(pz[:, 0:128], vtok[:], wsT[:, g, :])
                    S.tt("dve", zt[:], pz[:, 0:128], bsb[:, g * 128:(g + 1) * 128], ALU.add)
                    S.tt("pool", ao[:, g, c0:c0 + 128], zt[:], ug[:, c0:c0 + 128], ALU.mult)
            S.dma("pool", fm(st.AO, 0, 512, t0, t0 + n), ao[:, :, 0:n])
            lo0 = 1 if t0 == 0 else 0
            hi2 = n - 1 if t0 + n >= T else n
            for i in range(12):
                ps = proj(8 + i)
                S.ts("dve", ct[:, 0:n], ps[:, 1:n + 1], vc("bshift1", i), ALU.mult)
                S.stt(ct[:, lo0:n], ps[:, lo0:n], vc("bshift0", i), ct[:, lo0:n], ALU.mult, ALU.add)
                S.stt(rkv[:, i, 0:hi2], ps[:, 2:hi2 + 2], vc("bshift2", i), ct[:, 0:hi2], ALU.mult, ALU.add)
                if hi2 < n:
                    S.copy("dve", rkv[:, i, hi2:n], ct[:, hi2:n])
            ps = proj(20)
            S.act(lor[:, 0, 0:n], ps[:, 1:n + 1], AF.Tanh)
            ps = proj(21)
            S.copy("act", lor[:, 1, 0:n], ps[:, 1:n + 1])
            ps = proj(22)
            S.act(lor[:, 2, 0:n], ps[:, 1:n + 1], AF.Sigmoid)
            for d in range(2):
                for c in range(4):
                    ps = nb()
                    S.matmul(ps[:, 0:n], upb[:, d, c * 128:(c + 1) * 128], lor[:, 0, 0:n])
                    S.act(LDt[:, d * 4 + c, 0:n], ps[:, 0:n], AF.Sigmoid, bias=vc("bw0", d * 4 + c), scale=1.0)
                    ps = nb()
                    S.matmul(ps[:, 0:n], upb[:, 2 + d, c * 128:(c + 1) * 128], lor[:, 1, 0:n])
                    S.act(At[:, d * 4 + c, 0:n], ps[:, 0:n], AF.Sigmoid, bias=vc("ba0", d * 4 + c), scale=1.0)
            S.ts("pool", LDt[:, :, 0:n], LDt[:, :, 0:n], -float(np.exp(-0.5)), ALU.mult)
            for c in range(4):
                ps = nb()
                S.matmul(ps[:, 0:n], upb[:, 4, c * 128:(c + 1) * 128], lor[:, 2, 0:n])
                S.copy("act", Gt[:, c, 0:n], ps[:, 0:n])
            for c in range(4):
                S.ts("dve", kk[:, c, 0:n], rkv[:, 4 + c, 0:n], vc("bkk", c), ALU.mult)
            S.act(sq[:, 0:4, 0:n], kk[:, :, 0:n], AF.Square)
            for c in range(4):
                ps = nb()
                S.matmul(ps[:, 0:n], cc("blk64"), sq[:, c, 0:n])
                S.ts("dve", ct[:, 0:n], ps[:, 0:n], 1e-12, ALU.max)
                S.act(ct[:, 0:n], ct[:, 0:n], AF.Sqrt)
                S.op("dve", lambda: nc.vector.reciprocal(ct.t[:, 0:n], ct.t[:, 0:n]), reads=[ct[:]], writes=[ct[:]])
                S.tt("dve", kk[:, c, 0:n], kk[:, c, 0:n], ct[:, 0:n], ALU.mult)
            for d in range(2):
                for c in range(4):
                    S.ts("pool", kd[:, d, c, 0:n], At[:, d * 4 + c, 0:n], vc("bka", c), ALU.mult, omka[:, c:c + 1], ALU.add)
                S.tt("dve", kd[:, d, :, 0:n], kd[:, d, :, 0:n], rkv[:, 4:8, 0:n], ALU.mult)
                S.tt("pool", bb[:, d, :, 0:n], kk[:, :, 0:n], At[:, d * 4:d * 4 + 4, 0:n], ALU.mult)
                S.tt("dve", rk[:, d, :, 0:n], kd[:, d, :, 0:n], rkv[:, 0:4, 0:n], ALU.mult)
                for c in range(4):
                    S.ts("pool", rk[:, d, c, 0:n], rk[:, d, c, 0:n], vc("brk", c), ALU.mult)
            for c in range(4):
                ps = nb()
                for d in range(2):
                    S.matmul(ps[:, 0:n], cc("blk64"), rk[:, d, c, 0:n], start=(d == 0), stop=(d == 1))
                S.tt("dve", bv[:, c, 0:n], ps[:, 0:n], rkv[:, 8 + c, 0:n], ALU.mult)
            sl = (t0, t0 + n)
            S.dma("pool", fm(st.R, 0, 512, *sl), rkv[:, 0:4, 0:n])
            S.dma("pool", fm(st.VV, 0, 512, *sl), rkv[:, 8:12, 0:n])
            S.dma("pool", fm(st.KK, 0, 512, *sl), kk[:, :, 0:n])
            S.dma("pool", fm(st.GG, 0, 512, *sl), Gt[:, :, 0:n])
            S.dma("pool", fm(st.BV, 0, 512, *sl), bv[:, :, 0:n])
            for d in range(2):
                S.dma("pool", fm((st.KD0, st.KD1)[d], 0, 512, *sl), kd[:, d, :, 0:n])
                S.dma("pool", fm((st.B0, st.B1)[d], 0, 512, *sl), bb[:, d, :, 0:n])
                S.dma("pool", fm((st.LD0, st.LD1)[d], 0, 512, *sl), LDt[:, d * 4:d * 4 + 4, 0:n])
    S.pop()


def scan0_gen(G, d):
    S, nc, fm, vc, cc, nb, ew = G.S, G.nc, G.fm, G.vc, G.cc, G.nb, G.ew
    NCH = TSS // LR
    f4 = lambda nm: S.sbuf(f"s0_{nm}", [128, 4, TSS], F32)
    Rr, KDd, VV, KKk, Bd, LD = f4("R"), f4("KD"), f4("VV"), f4("KK"), f4("B"), f4("LD")
    f4b = lambda nm: S.sbuf(f"s0_{nm}", [128, 4, TSS], BF16)
    lg, t1, kbar, bbar = f4("lg"), f4("t1"), f4("kbar"), f4("bbar")
    kkt, khat, bhat, rt = f4b("kkt"), f4b("khat"), f4b("bhat"), f4b("rt")
    gg = f4("g")
    yt = f4("y")
    fbd = lambda nm: S.sbuf(f"s0_{nm}", [128, 4, NCH, 128], BF16)
    kkt_bd, bhat_bd, rt_bd = fbd("kktbd"), fbd("bhatbd"), fbd("rtbd")
    vtok = S.sbuf("s0_vtok", [64, NCH, 512], BF16)
    kbtok = S.sbuf("s0_kbtok", [64, NCH, 512], BF16)
    bbtok = S.sbuf("s0_bbtok", [64, NCH, 512], BF16)
    Hbb = S.sbuf("s0_Hb", [128, 4, 128], BF16)
    Rmb = S.sbuf("s0_Rmb", [64, 512], BF16)
    Hbd = S.sbuf("s0_H", [128, 4, 128], F32)
    mk = lambda nm: S.sbuf(f"s0_{nm}", [64, 512], BF16)
    AkkT, ArkT, ArbT, Wsb, Un = [mk(x) for x in ("AkkT", "ArkT", "ArbT", "Wsb", "Un")]
    Pa, Pta, Pb, Ptb = [S.sbuf(f"s0_{x}", [64, 512], F32) for x in ("Pa", "Pta", "Pb", "Ptb")]
    Rm = S.sbuf("s0_Rm", [64, 512], F32)
    S.memset("dve", Hbd[:], 0.0)
    S.memset("dve", Hbb[:], 0.0)
    S.memset("pool", kkt_bd[:], 0.0)
    S.memset("pool", bhat_bd[:], 0.0)
    S.memset("pool", rt_bd[:], 0.0)
    strict = cc("su", 64) if d == 0 else cc("sl", 64)
    strictT = cc("sl", 64) if d == 0 else cc("su", 64)
    incl = cc("iu", 64) if d == 0 else cc("il", 64)
    id8 = cc("id8", 64)

    def blk(buf, h):
        return buf[0:64, h * 64:(h + 1) * 64]

    def mmd(v):
        if not INV_FP32R:
            return v

        return V(v.ap.bitcast(mybir.dt.float32r), v.keys)

    def to_bd(dst, src, nch, n):
        for hp in range(2):
            rows = slice(hp * 64, hp * 64 + 64)
            S.copy("pool" if hp else "dve", dst.v(dst.t[rows, :, 0:nch, hp * 64:hp * 64 + 64]),
                   src.v(src.t[rows, :, 0:n].rearrange("p c (k l) -> p c k l", l=LR)))

    for st in (G.ctx, G.lat):
        T = st.T
        tl = [(t0, min(TSS, T - t0)) for t0 in range(0, T, TSS)]
        if d == 1:
            tl = tl[::-1]
        for (t0, n) in tl:
            sl = (t0, t0 + n)
            for dst, src in ((Rr, st.R), (KDd, (st.KD0, st.KD1)[d]), (VV, st.VV), (KKk, st.KK), (Bd, (st.B0, st.B1)[d]), (LD, (st.LD0, st.LD1)[d])):
                S.dma("sp", dst[:, :, 0:n], fm(src, 0, 512, *sl))
            nch = n // LR
            for c in range(4):
                S.op("dve", lambda c=c: nc.vector.tensor_tensor_scan(lg.t[:, c, 0:n], cc("seg").ap[:, 0:n], LD.t[:, c, 0:n], 0.0, ALU.mult, ALU.add),
                     reads=[cc("seg"), LD[:]], writes=[lg[:]])
            lg4 = lambda b: b.t[:, :, 0:n].rearrange("p c (k l) -> p c k l", l=LR)
            if d == 1:
                tot = lg.v(lg4(lg)[:, :, :, LR - 1:LR].broadcast_to([128, 4, nch, LR]))
                S.tt("dve", t1.v(lg4(t1)), tot, lg.v(lg4(lg)), ALU.subtract)
                S.tt("dve", lg[:, :, 0:n], t1[:, :, 0:n], LD[:, :, 0:n], ALU.add)
            ie = (LR - 1) if d == 0 else 0
            S.act(gg[:, :, 0:n], lg[:, :, 0:n], AF.Exp)
            S.tt("pool", rt[:, :, 0:n], Rr[:, :, 0:n], gg[:, :, 0:n], ALU.mult)
            S.act(t1[:, :, 0:n], lg[:, :, 0:n], AF.Exp, scale=-1.0)
            S.tt("dve", khat[:, :, 0:n], KDd[:, :, 0:n], t1[:, :, 0:n], ALU.mult)
            S.tt("pool", bhat[:, :, 0:n], Bd[:, :, 0:n], t1[:, :, 0:n], ALU.mult)
            S.tt("dve", t1[:, :, 0:n], lg[:, :, 0:n], LD[:, :, 0:n], ALU.subtract)
            S.act(t1[:, :, 0:n], t1[:, :, 0:n], AF.Exp)
            S.tt("dve", kkt[:, :, 0:n], KKk[:, :, 0:n], t1[:, :, 0:n], ALU.mult)
            lgL = lg.v(lg4(lg)[:, :, :, ie:ie + 1].broadcast_to([128, 4, nch, LR]))
            S.tt("dve", t1.v(lg4(t1)), lgL, lg.v(lg4(lg)), ALU.subtract)
            S.act(t1[:, :, 0:n], t1[:, :, 0:n], AF.Exp)
            S.tt("dve", kbar[:, :, 0:n], KDd[:, :, 0:n], t1[:, :, 0:n], ALU.mult)
            S.tt("pool", bbar[:, :, 0:n], Bd[:, :, 0:n], t1[:, :, 0:n], ALU.mult)
            to_bd(kkt_bd, kkt, nch, n)
            to_bd(bhat_bd, bhat, nch, n)
            to_bd(rt_bd, rt, nch, n)
            for ch in range(nch):
                for src, dst in ((VV, vtok), (kbar, kbtok), (bbar, bbtok)):
                    ps = nb()
                    for c in range(4):
                        S.transpose(ps[0:64, c * 128:(c + 1) * 128], src[:, c, ch * LR:(ch + 1) * LR], cc("ident"))
                    S.copy("act", dst[:, ch, :], ps[0:64, :])
            chs = list(range(nch))
            if d == 1:
                chs = chs[::-1]
            for ch in chs:
                c0 = ch * LR
                pl = lambda buf, pr: buf[:, pr, c0:c0 + LR]
                bdv = lambda buf, pr: buf[:, pr, ch, :]
                pp = lambda ps, pr: ps[0:64, pr * 128:(pr + 1) * 128]
                psN, psNt = nb(), nb()
                for pr in range(4):
                    S.matmul(pp(psN, pr), pl(bhat, pr), bdv(kkt_bd, pr))
                    S.matmul(pp(psNt, pr), pl(kkt, pr), bdv(bhat_bd, pr))
                S.tt("dve", Pa[:], psN[0:64, :], strict, ALU.mult)
                S.tt("dve", Pta[:], psNt[0:64, :], strictT, ALU.mult)
                psKK, psRK, psRB = nb(), nb(), nb()
                for pr in range(4):
                    S.matmul(pp(psKK, pr), pl(khat, pr), bdv(kkt_bd, pr))
                    S.matmul(pp(psRK, pr), pl(khat, pr), bdv(rt_bd, pr))
                    S.matmul(pp(psRB, pr), pl(bhat, pr), bdv(rt_bd, pr))
                S.tt("dve", AkkT[:], psKK[0:64, :], strict, ALU.mult)
                S.tt("dve", ArkT[:], psRK[0:64, :], incl, ALU.mult)
                S.tt("dve", ArbT[:], psRB[0:64, :], incl, ALU.mult)
                S.tt("pool", Rm[:], id8, Pa[:], ALU.subtract)
                P, Pt, P2, Pt2 = Pa, Pta, Pb, Ptb
                for lev in range(5):
                    ps1, ps2 = nb(), nb()
                    for h in range(8):
                        if lev < 4:
                            S.matmul(blk(ps1, h), mmd(blk(Pt, h)), mmd(blk(P, h)))
                        S.matmul(blk(ps2, h), mmd(blk(P, h)), mmd(blk(Pt, h)))
                    if lev < 4:
                        S.copy("act", P2[:], ps1[0:64, :])
                    S.copy("dve", Pt2[:], ps2[0:64, :])
                    ps3 = nb()
                    for h in range(8):
                        S.matmul(blk(ps3, h), mmd(blk(Pt2, h)), mmd(blk(Rm, h)))
                    S.tt("dve", Rm[:], Rm[:], ps3[0:64, :], ALU.add)
                    if lev == 4:
                        S.copy("pool", Rmb[:], Rm[:])
                    P, Pt, P2, Pt2 = P2, Pt2, P, Pt
                    if lev in (1, 3):
                        yield
                psW = nb()
                for pr in range(4):
                    S.matmul(pp(psW, pr), pl(kkt, pr), Hbb[:, pr, :], start=True, stop=False)
                    for hp in range(2):
                        h = 2 * pr + hp
                        S.matmul(blk(psW, h), blk(AkkT, h), vtok[:, ch, h * 64:(h + 1) * 64], start=False, stop=(hp == 1))
                S.copy("act", Wsb[:], psW[0:64, :])
                psU = nb()
                for h in range(8):
                    S.matmul(blk(psU, h), blk(Rmb, h), blk(Wsb, h))
                S.ts("dve", Un[:], psU[0:64, :], -1.0, ALU.mult)
                psY = nb()
                psH = nb()
                for pr in range(4):
                    S.matmul(psY[:, pr * 64:(pr + 1) * 64], Hbb[:, pr, :], pl(rt, pr), start=True, stop=False)
                    for hp in range(2):
                        h = 2 * pr + hp
                        oy = psY[hp * 64:(hp + 1) * 64, pr * 64:(pr + 1) * 64]
                        S.matmul(oy, vtok[:, ch, h * 64:(h + 1) * 64], blk(ArkT, h), start=False, stop=False)
                        S.matmul(oy, blk(Un, h), blk(ArbT, h), start=False, stop=True)
                for pr in range(4):
                    oh = psH[:, pr * 128:(pr + 1) * 128]
                    S.matmul(oh, kbtok[:, ch, pr * 128:(pr + 1) * 128], vtok[:, ch, pr * 128:(pr + 1) * 128], start=True, stop=False)
                    S.matmul(oh, bbtok[:, ch, pr * 128:(pr + 1) * 128], Un[0:64, pr * 128:(pr + 1) * 128], start=False, stop=True)
                S.copy("act", yt.v(yt.t[:, :, c0:c0 + LR]), psY.v(psY.t[:, 0:256].rearrange("p (c t) -> p c t", t=64)))
                for pr in range(4):
                    for hp in range(2):
                        h = 2 * pr + hp
                        rows = slice(hp * 64, hp * 64 + 64)
                        S.stt(Hbd[rows, pr, hp * 64:hp * 64 + 64], Hbd[rows, pr, hp * 64:hp * 64 + 64], gg[rows, pr, c0 + ie:c0 + ie + 1],
                              psH[rows, h * 64:(h + 1) * 64], ALU.mult, ALU.add, e="dve")
                S.copy("pool", Hbb[:], Hbd[:])
                yield
            S.dma("pool", fm((st.Y0, st.Y1)[d], 0, 512, *sl), yt[:, :, 0:n])


TSS = 128
INV_FP32R = False


def interleave(gens):
    gens = list(gens)
    while gens:
        for g in list(gens):
            try:
                next(g)
            except StopIteration:
                gens.remove(g)


def scan0_both(G):
    G.S.push()
    interleave([scan0_gen(G, 0), scan0_gen(G, 1)])
    G.S.pop()


def out_phase(G, l):
    S, nc, fm, vc, cc, nb, ew = G.S, G.nc, G.fm, G.vc, G.cc, G.nb, G.ew
    W = G.w
    S.push()
    wo = S.sbuf("o_wo", [128, KD, D], BF16)
    wu = S.sbuf("o_wu", [128, KD, 2 * DFF], BF16)
    stage = [S.sbuf(f"o_st{i}", [128, KD, 256], F32) for i in range(2)]
    wsrc = W["ab_w_out"] if l == 0 else W["c_w_out"]
    G.load_w_bf16(wo, lambda c0, c1: wsrc.v(wsrc.t[:, c0:c1].rearrange("(j p) f -> p j f", p=128)), D, stage)
    fu = W["ffn_up"]
    G.load_w_bf16(wu, lambda c0, c1: fu.v(fu.t[l, :, c0:c1].rearrange("(j p) f -> p j f", p=128)), 2 * DFF, stage)
    f8 = lambda nm: S.sbuf(f"o_{nm}", [128, KD, TS], F32)
    xin = f8("xin")
    mo = S.sbuf("o_mo", [128, KD, TS], BF16)
    hf = S.sbuf("o_hf", [128, KD, TS], BF16)
    rstd = S.sbuf("o_rstd", [128, TS], F32)
    tmp = S.sbuf("o_tmp", [128, TS], F32)
    gcm = S.sbuf("o_gcm", [128, 8], F32)
    cff = S.sbuf("o_cff", [128, 8], F32)
    fab = [S.sbuf(f"o_fab{i}", [128, 2, TS], F32) for i in range(2)]
    faa = [S.sbuf(f"o_faa{i}", [128, 2, TS], BF16) for i in range(2)]
    a1, a2, a3, a4 = f8("a1"), f8("a2"), f8("a3"), f8("a4")
    ymix, sq, x1 = a2, a3, a4
    streams = (G.ctx, G.lat) if l == 0 else (G.lat,)
    for st in streams:
        T = st.T
        G.make_gcoef(l, st, f"nmo{l}", 2, gcm[:])
        G.make_coef(l, st, f"nfp{l}", 4, cff[:])
        for (t0, n) in G.tiles(T):
            sl = (t0, t0 + n)
            S.dma("sp", xin[:, :, 0:n], fm(st.res[2 * l], 0, D, *sl))
            if l == 0:
                S.dma("sp", mo[:, 0:4, 0:n], fm(st.AO, 0, 512, *sl))
                y0, y1, gt, bvt = a1, a2, a3, a4
                S.dma("sp", y0[:, 0:4, 0:n], fm(st.Y0, 0, 512, *sl))
                S.dma("sp", y1[:, 0:4, 0:n], fm(st.Y1, 0, 512, *sl))
                S.dma("sp", gt[:, 0:4, 0:n], fm(st.GG, 0, 512, *sl))
                S.dma("sp", bvt[:, 0:4, 0:n], fm(st.BV, 0, 512, *sl))
                S.tt("dve", y0[:, 0:4, 0:n], y0[:, 0:4, 0:n], y1[:, 0:4, 0:n], ALU.add)
                for c in range(4):
                    ps = nb()
                    S.matmul(ps[:, 0:n], cc("blk64"), y0[:, c, 0:n])
                    S.stt(y0[:, c, 0:n], ps[:, 0:n], -1.0 / 64, y0[:, c, 0:n], ALU.mult, ALU.add)
                    S.act(y1[:, c, 0:n], y0[:, c, 0:n], AF.Square)
                    G.rstd_bcast([y1[:, c, 0:n]], n, rstd[:, 0:n], cc("blk64"), 1.0 / 64, LN_EPS, tmp[:, 0:n])
                    S.tt("dve", y0[:, c, 0:n], y0[:, c, 0:n], rstd[:, 0:n], ALU.mult)
                    S.ts("dve", y0[:, c, 0:n], y0[:, c, 0:n], vc("blng", c), ALU.mult, vc("blnb", c), ALU.add)
                S.tt("dve", y0[:, 0:4, 0:n], y0[:, 0:4, 0:n], bvt[:, 0:4, 0:n], ALU.add)
                S.tt("dve", mo[:, 4:8, 0:n], y0[:, 0:4, 0:n], gt[:, 0:4, 0:n], ALU.mult)
            else:
                h0, h1, xcv, sz = a1, a2, a3, a4
                S.dma("sp", h0[:, :, 0:n], fm(st.H0, 0, D, *sl))
                S.dma("sp", h1[:, :, 0:n], fm(st.H1, 0, D, *sl))
                S.dma("sp", xcv[:, :, 0:n], fm(st.XCV, 0, D, *sl))
                S.dma("sp", sz[:, :, 0:n], fm(st.SZ, 0, D, *sl))
                S.tt("dve", h0[:, :, 0:n], h0[:, :, 0:n], h1[:, :, 0:n], ALU.add)
                S.act(h1[:, :, 0:n], h0[:, :, 0:n], AF.Square)
                for hd in range(4):
                    G.rstd_bcast([h1[:, 2 * hd, 0:n], h1[:, 2 * hd + 1, 0:n]], n, rstd[:, 0:n], cc("ones"), 1.0 / 256, EPS, tmp[:, 0:n])
                    for j in (2 * hd, 2 * hd + 1):
                        S.stt(h0[:, j, 0:n], h0[:, j, 0:n], vc("cnorm", j), rstd[:, 0:n], ALU.mult, ALU.mult)
                        S.stt(h0[:, j, 0:n], xcv[:, j, 0:n], vc("cskip", j), h0[:, j, 0:n], ALU.mult, ALU.add)
                S.tt("dve", mo[:, :, 0:n], h0[:, :, 0:n], sz[:, :, 0:n], ALU.mult)
            for oc in range(KD):
                ps = nb()
                for k in range(KD):
                    S.matmul(ps[:, 0:n], wo[:, k, oc * 128:(oc + 1) * 128], mo[:, k, 0:n], start=(k == 0), stop=(k == KD - 1))
                S.copy("act", ymix[:, oc, 0:n], ps[:, 0:n])
            G.post_norm_res(ymix, n, gcm, xin, x1, sq, rstd, tmp, sqb=hf)
            S.dma("pool", fm(st.res[2 * l + 1], 0, D, *sl), x1[:, :, 0:n])
            G.norm_mod(x1, n, cff, 3, st, hf, sq, rstd, tmp)
            for og in range(NF):
                fb = (faa if og < NF // 2 else fab)[og % 2]
                for o2 in range(2):
                    oc = og * 2 + o2
                    ps = nb()
                    for k in range(KD):
                        S.matmul(ps[:, 0:n], wu[:, k, oc * 128:(oc + 1) * 128], hf[:, k, 0:n], start=(k == 0), stop=(k == KD - 1))
                    S.copy("act" if oc % 2 else "dve", fb[:, o2, 0:n], ps[:, 0:n])
                dstb = st.FA if og < NF // 2 else st.FB
                r0 = (og * 2 % NF) * 128
                S.dma("pool", fm(dstb, r0, r0 + 256, *sl), fb[:, :, 0:n])
    S.pop()


def ffn2_phase(G, l):
    S, nc, fm, vc, cc, nb, ew = G.S, G.nc, G.fm, G.vc, G.cc, G.nb, G.ew
    W = G.w
    S.push()
    GW = 64
    wd = S.sbuf("f_wd", [128, NF, D], BF16)
    at = S.sbuf("f_at", [128, NF, TS + 2 * GW + 2], BF16)
    S.memset("pool", at[:], 0.0)
    GP = GW + 1
    nfc = S.sbuf("f_nfc", [128, 9 * NF], F32)
    S.ts("dve", nfc[:], vc(f"fconv{l}_0", 0, 9 * NF), -1.0, ALU.mult)
    bt = S.sbuf("f_bt", [128, NF, TS], F32)
    xin0 = S.sbuf("f_xin0", [128, KD, TS], F32)
    stage = [bt[:, :, 0:256], xin0.v(xin0.t[:].rearrange("p a b -> p (a b)")[:, 0:NF * 64].rearrange("p (a b) -> p a b", b=64))]
    dg = S.sbuf("f_dg", [128, NF * 9, 128], BF16)
    for j in range(NF):
        for ti in range(9):
            if ti % 2:
                S.act(dg[:, j * 9 + ti, :], cc("ident"), AF.Copy, scale=vc(f"fconv{l}_{ti}", j))
            else:
                S.ts("dve", dg[:, j * 9 + ti, :], cc("ident"), vc(f"fconv{l}_{ti}", j), ALU.mult)
    fd = W["ffn_down"]
    c0 = 0
    i = 0
    while c0 < D:
        stg = stage[0]
        S.dma("sp", stg, fd.v(fd.t[l, :, c0:c0 + 256].rearrange("(j p) f -> p j f", p=128)))
        S.copy("dve" if i % 2 else "act", wd[:, :, c0:c0 + 256], stg)
        c0 += 256
        i += 1
    cv_2 = [S.sbuf("f_cv%d" % i, [128, TS], F32) for i in range(3)]
    gb = S.sbuf("f_gb", [128, NF, TS], BF16)
    f8 = lambda nm: S.sbuf(f"f_{nm}", [128, KD, TS], F32)
    xin, yf, sq = xin0, f8("yf"), f8("sq")
    xo = yf
    rstd = S.sbuf("f_rstd", [128, TS], F32)
    tmp = S.sbuf("f_tmp", [128, TS], F32)
    gcf = S.sbuf("f_gcf", [128, 8], F32)
    streams = (G.ctx, G.lat) if l == 0 else (G.lat,)
    for st in streams:
        T = st.T
        G.make_gcoef(l, st, f"nfo{l}", 5, gcf[:])
        isl = st is G.lat
        for (t0, n) in G.tiles(T):
            sl = (t0, t0 + n)
            if isl:
                Wd, R = GW, n // GW
                lo, hi = t0 - GW, t0 + n + GW
                clo, chi = max(lo, 0), min(hi, T)
                if lo < 0:
                    S.memset("pool", at[:, :, 1:1 + GW], 0.0)
                if hi > T:
                    S.memset("pool", at[:, :, GP + n:GP + n + GW], 0.0)
                S.dma("sp", at[:, :, 1 + clo - lo:1 + chi - lo], fm(st.FA, 0, DFF, clo, chi))
                taps = [(dr, dc) for dr in (-1, 0, 1) for dc in (-1, 0, 1)]
            else:
                assert T <= TS
                Wd, R = n, 1
                S.memset("pool", at[:, :, GP - 1:GP], 0.0)
                S.memset("pool", at[:, :, GP + n:GP + n + 1], 0.0)
                S.dma("sp", at[:, :, GP:GP + n], fm(st.FA, 0, DFF, t0, t0 + n))
                taps = [(0, -1), (0, 0), (0, 1)]
            S.dma("sp", bt[:, :, 0:n], fm(st.FB, 0, DFF, *sl))
            S.dma("sp", xin[:, :, 0:n], fm(st.res[2 * l + 1], 0, D, *sl))
            for j in range(NF):
                cv = cv_2[j % 3]

                pc = nb()
                order = [(0, 0)] + [t_ for t_ in taps if t_ != (0, 0)]
                for ti_, (dr, dc) in enumerate(order):
                    w_ = dg[:, j * 9 + (dr + 1) * 3 + (dc + 1), :]
                    b0 = GP + dr * Wd + dc
                    S.matmul(pc[:, 0:n], w_, at[:, j, b0:b0 + n], start=(ti_ == 0), stop=(ti_ == len(order) - 1))
                if isl:
                    for dr in (-1, 0, 1):
                        for dc in (-1, 1):
                            col = 0 if dc == -1 else Wd - 1
                            a0 = GP + dr * Wd + dc + col
                            a_ = at.v(at.t[:, j, a0:a0 + (R - 1) * Wd + 1:Wd], j)
                            o_ = pc.v(pc.t[:, col:col + (R - 1) * Wd + 1:Wd])
                            ti = (dr + 1) * 3 + (dc + 1)
                            S.stt(o_, a_, nfc[:, ti * NF + j:ti * NF + j + 1], o_, ALU.mult, ALU.add)
                S.act(cv[:, 0:n], pc[:, 0:n], AF.Gelu_apprx_tanh, bias=vc(f"fcb{l}", j), scale=1.0)
                S.tt("dve" if j % 2 else "pool", gb[:, j, 0:n], cv[:, 0:n], bt[:, j, 0:n], ALU.mult)
            for oc in range(KD):
                ps = nb()
                for k in range(NF):
                    S.matmul(ps[:, 0:n], wd[:, k, oc * 128:(oc + 1) * 128], gb[:, k, 0:n], start=(k == 0), stop=(k == NF - 1))
                S.copy("act", yf[:, oc, 0:n], ps[:, 0:n])
            G.post_norm_res(yf, n, gcf, xin, xo, sq, rstd, tmp, sqb=gb)
            S.dma("pool", fm(st.res[2 * l + 2], 0, D, *sl), xo[:, :, 0:n])
    S.pop()


def in1_phase(G):
    S, nc, fm, vc, cc, nb, ew = G.S, G.nc, G.fm, G.vc, G.cc, G.nb, G.ew
    W = G.w
    S.push()
    NH = TS + 2
    wb = S.sbuf("i1_wb", [128, KD, 2064], BF16)
    stage = [S.sbuf(f"i1_st{i}", [128, KD, 512], F32) for i in range(2)]
    G.load_w_bf16(wb, lambda c0, c1: W["c_w_in"].v(W["c_w_in"].t[:, c0:c1].rearrange("(j p) f -> p j f", p=128)), 2064, stage)
    bdf = S.sbuf("i1_bdf", [128, 3, KD, 128], F32)
    S.dma("sp", bdf[:], W["c_bd"][:])
    coef = S.sbuf("i1_coef", [128, 8], F32)
    xt = S.sbuf("i1_xt", [128, KD, NH], F32)
    sq = S.sbuf("i1_sq", [128, KD, NH], F32)
    rstd = S.sbuf("i1_rstd", [128, NH], F32)
    tmp = S.sbuf("i1_tmp", [128, NH], F32)
    h = S.sbuf("i1_h", [128, KD, NH], BF16)
    xm = S.sbuf("i1_xm", [128, KD, NH], F32)
    xcv = S.sbuf("i1_xcv", [128, KD, TS], F32)
    szt = S.sbuf("i1_sz", [128, KD, TS], F32)
    qt = S.sbuf("i1_q", [128, KD, TS], F32)
    kt = S.sbuf("i1_k", [128, KD, TS], F32)
    ktok = S.sbuf("i1_ktok", [128, TS // 128, D], F32)
    vtok = S.sbuf("i1_vtok", [128, TS // 128, D], F32)
    gat = S.sbuf("i1_gat", [16, TS], F32)
    ct = S.sbuf("i1_ct", [128, TS], F32)
    for st in (G.ctx, G.lat):
        T = st.T
        G.make_coef(1, st, "nmp1", 1, coef[:])
        for (t0, n) in G.tiles(T):
            nh = n + 2
            sl = (t0, t0 + n)
            G.load_halo(xt, st.res[2], t0, n, T, 0, D)
            G.norm_mod(xt, nh, coef, 0, st, h, sq, rstd, tmp)

            def proj(oc, msz=128):
                ps = nb()
                for k in range(KD):
                    S.matmul(ps[0:msz, 0:nh], wb[:, k, oc * 128:oc * 128 + msz], h[:, k, 0:nh], start=(k == 0), stop=(k == KD - 1))
                return ps
            for j in range(KD):
                ps = proj(j)
                S.copy("act", xm[:, j, 0:nh], ps[:, 0:nh])
            if t0 == 0:
                S.memset("pool", xm[:, :, 0:1], 0.0)
            if t0 + n >= T:
                S.memset("pool", xm[:, :, n + 1:n + 2], 0.0)
            for j in range(KD):
                ps = proj(KD + j)
                S.act(szt[:, j, 0:n], ps[:, 1:n + 1], AF.Sigmoid)
            ps = proj(2 * KD, 16)
            S.act(gat[0:16, 0:n], ps[0:16, 1:n + 1], AF.Identity, bias=G.vcr("cgb", 16), scale=1.0)
            S.dma("pool", st.GATE[:, t0:t0 + n], gat[:, 0:n])
            for j in range(KD):
                S.ts("dve", ct[:, 0:n], xm[:, j, 1:n + 1], vc("cconv1", j), ALU.mult, vc("ccb", j), ALU.add)
                S.stt(ct[:, 0:n], xm[:, j, 0:n], vc("cconv0", j), ct[:, 0:n], ALU.mult, ALU.add)
                S.stt(ct[:, 0:n], xm[:, j, 2:n + 2], vc("cconv2", j), ct[:, 0:n], ALU.mult, ALU.add)
                S.act(xcv[:, j, 0:n], ct[:, 0:n], AF.Silu)
            for j in range(KD):
                ps = nb()
                S.matmul(ps[:, 0:n], bdf[:, 0, j, :], xcv[:, j, 0:n])
                S.copy("act", qt[:, j, 0:n], ps[:, 0:n])
                ps = nb()
                S.matmul(ps[:, 0:n], bdf[:, 1, j, :], xcv[:, j, 0:n])
                S.ts("dve", kt[:, j, 0:n], ps[:, 0:n], 1.0 / 16, ALU.mult)
                for cch in range(n // 128):
                    ps = nb()
                    S.matmul(ps[:, 0:128], xcv[:, j, cch * 128:(cch + 1) * 128], bdf[:, 1, j, :])
                    S.ts("dve", ktok[:, cch, j * 128:(j + 1) * 128], ps[:, 0:128], 1.0 / 16, ALU.mult)
                    ps = nb()
                    S.matmul(ps[:, 0:128], xm[:, j, 1 + cch * 128:1 + (cch + 1) * 128], bdf[:, 2, j, :])
                    S.copy("act", vtok[:, cch, j * 128:(j + 1) * 128], ps[:, 0:128])
            S.dma("pool", fm(st.MQ, 0, D, *sl), qt[:, :, 0:n])
            S.dma("pool", fm(st.MK, 0, D, *sl), kt[:, :, 0:n])
            S.dma("pool", fm(st.XCV, 0, D, *sl), xcv[:, :, 0:n])
            S.dma("pool", fm(st.SZ, 0, D, *sl), szt[:, :, 0:n])
            for cch in range(n // 128):
                S.dma("pool", st.KTOK[t0 + cch * 128:t0 + (cch + 1) * 128, :], ktok[:, cch, :])
                S.dma("pool", st.VTOK[t0 + cch * 128:t0 + (cch + 1) * 128, :], vtok[:, cch, :])
    S.pop()


def scan1_gen(G, d, h0):
    S, nc, fm, vc, cc, nb, ew = G.S, G.nc, G.fm, G.vc, G.cc, G.nb, G.ew
    L = LM
    NHD = 2
    qtf = S.sbuf("s1_qf", [128, 2 * NHD, L], F32)
    ktf = S.sbuf("s1_kf", [128, 2 * NHD, L], F32)
    ktok = S.sbuf("s1_ktok", [128, NHD * 256], F32)
    vtokf = S.sbuf("s1_vtokf", [128, NHD * 256], F32)
    qt = S.sbuf("s1_q", [128, 2 * NHD, L], BF16)
    kt = S.sbuf("s1_k", [128, 2 * NHD, L], BF16)
    vtok = S.sbuf("s1_vtok", [128, NHD * 256], BF16)
    C0b = S.sbuf("s1_C0b", [128, NHD, 2, 256], BF16)
    n0b = S.sbuf("s1_n0b", [128, NHD, 2, 128], BF16)
    onesb = S.sbuf("s1_onesb", [128, 128], BF16)
    S.memset("dve", C0b[:], 0.0)
    S.memset("dve", n0b[:], 0.0)
    S.memset("dve", onesb[:], 1.0)
    g16 = S.sbuf("s1_g16", [16, L], F32)
    lf = S.sbuf("s1_lf", [16, L], F32)
    b16 = S.sbuf("s1_b16", [16, L], F32)
    t16 = S.sbuf("s1_t16", [16, L], F32)
    T1 = S.sbuf("s1_T1", [128, 16], F32)
    T2 = S.sbuf("s1_T2", [128, 16], F32)
    CT = S.sbuf("s1_CT", [128, 4], F32)
    Bbs = S.sbuf("s1_Bbs", [128, L], F32)
    EQb = S.sbuf("s1_EQb", [128, L], F32)
    Dm = S.sbuf("s1_Dm", [128, L], F32)
    STs = S.sbuf("s1_STs", [128, L], BF16)
    ekb = S.sbuf("s1_ekb", [128, 1], F32)
    Kbar = S.sbuf("s1_Kbar", [128, 256], BF16)
    C0 = S.sbuf("s1_C0", [128, NHD, 2, 256], F32)
    n0 = S.sbuf("s1_n0", [128, NHD, 2, 128], F32)
    den = S.sbuf("s1_den", [128, L], F32)
    num = S.sbuf("s1_num", [128, L], F32)
    ht = S.sbuf("s1_h", [128, 2 * NHD, L], F32)
    S.memset("dve", C0[:], 0.0)
    S.memset("pool", n0[:], 0.0)
    mask = cc("mu") if d == 0 else cc("ml")
    iL = (L - 1) if d == 0 else 0
    for st in (G.ctx, G.lat):
        T = st.T
        chunks = list(range(0, T, L))
        if d == 1:
            chunks = chunks[::-1]
        for t0 in chunks:
            sl = (t0, t0 + L)
            r0, r1 = h0 * 256, (h0 + NHD) * 256
            S.dma("sp", qtf[:], fm(st.MQ, r0, r1, *sl))
            S.dma("sp", ktf[:], fm(st.MK, r0, r1, *sl))
            S.dma("sp", ktok[:], st.KTOK[t0:t0 + L, r0:r1])
            S.dma("sp", vtokf[:], st.VTOK[t0:t0 + L, r0:r1])
            S.copy("dve", qt[:], qtf[:])
            S.copy("pool", kt[:], ktf[:])
            S.copy("act", vtok[:], vtokf[:])
            S.dma("sp", g16[:], st.GATE[:, t0:t0 + L])
            S.act(lf[:], g16[:], AF.Sigmoid)
            S.act(lf[:], lf[:], AF.Ln)
            S.op("dve", lambda: nc.vector.tensor_tensor_scan(b16.t[:, :], cc("ones", 16, 0, L).ap, lf.t[:, :], 0.0, ALU.mult, ALU.add),
                 reads=[cc("ones"), lf[:]], writes=[b16[:]])
            if d == 1:
                S.tt("dve", t16[:], b16.v(b16.t[:, L - 1:L].broadcast_to([16, L])), b16[:], ALU.subtract)
                S.tt("dve", b16[:], t16[:], lf[:], ALU.add)
            p1 = nb()
            S.transpose(p1[:, 0:16], g16[:], cc("ident", 16, 0, 16))
            S.copy("act", T1[:], p1[:, 0:16])
            p2 = nb()
            S.transpose(p2[:, 0:16], b16[:], cc("ident", 16, 0, 16))
            S.copy("act", T2[:], p2[:, 0:16])
            S.tt("dve", CT[:], T1[:, 4 * d:4 * d + 4], T2[:, 8 + 4 * d:8 + 4 * d + 4], ALU.subtract)
            for h in range(NHD):
                r = 8 + 4 * d + h0 + h
                hg = h0 + h
                psB = nb()
                S.matmul(psB[:, 0:L], cc("sel", 16, r * 128, 128), b16[:])
                S.copy("act", Bbs[:], psB[:, 0:L])
                S.act(EQb[:], psB[:, 0:L], AF.Exp)
                S.act(Dm[:], Bbs[:], AF.Exp, bias=CT[:, hg:hg + 1], scale=1.0)
                S.tt("pool", Dm[:], Dm[:], mask, ALU.mult)
                psS = nb()
                for kc in range(2):
                    S.matmul(psS[:, 0:L], kt[:, 2 * h + kc, :], qt[:, 2 * h + kc, :], start=(kc == 0), stop=(kc == 1))
                S.tt("dve", STs[:], psS[:, 0:L], Dm[:], ALU.mult)
                psD = nb()
                S.matmul(psD[:, 0:L], onesb[:], STs[:])
                psE = nb()
                for kc in range(2):
                    S.matmul(psE[:, 0:L], n0b[:, h, kc, :], qt[:, 2 * h + kc, :], start=(kc == 0), stop=(kc == 1))
                S.tt("dve", den[:], psE[:, 0:L], EQb[:], ALU.mult)
                S.tt("dve", den[:], den[:], psD[:, 0:L], ALU.add)
                S.act(den[:], den[:], AF.Abs)
                S.ts("dve", den[:], den[:], 1.0, ALU.max)
                S.op("dve", lambda: nc.vector.reciprocal(den.t[:, :], den.t[:, :]), reads=[den[:]], writes=[den[:]])
                yield
                for vcx in range(2):
                    psI = nb()
                    S.matmul(psI[:, 0:L], vtok[:, h * 256 + vcx * 128:h * 256 + (vcx + 1) * 128], STs[:])
                    psX = nb()
                    for kc in range(2):
                        S.matmul(psX[:, 0:L], C0b[:, h, kc, vcx * 128:(vcx + 1) * 128], qt[:, 2 * h + kc, :], start=(kc == 0), stop=(kc == 1))
                    S.tt("dve", num[:], psX[:, 0:L], EQb[:], ALU.mult)
                    S.tt("dve", num[:], num[:], psI[:, 0:L], ALU.add)
                    S.tt("pool", ht[:, 2 * h + vcx, :], num[:], den[:], ALU.mult)
                S.act(ekb[:], CT[:, hg:hg + 1], AF.Exp, bias=Bbs[:, iL:iL + 1], scale=1.0)
                S.ts("dve", Kbar[:], ktok[:, h * 256:(h + 1) * 256], ekb[:, 0:1], ALU.mult)
                for kc in range(2):
                    psC = nb()
                    S.matmul(psC[:, 0:256], Kbar[:, kc * 128:(kc + 1) * 128], vtok[:, h * 256:(h + 1) * 256])
                    S.stt(C0[:, h, kc, :], C0[:, h, kc, :], EQb[:, iL:iL + 1], psC[:, 0:256], ALU.mult, ALU.add)
                    S.copy("act", C0b[:, h, kc, :], C0[:, h, kc, :])
                    psN = nb()
                    S.matmul(psN[:, 0:128], Kbar[:, kc * 128:(kc + 1) * 128], onesb[:])
                    S.stt(n0[:, h, kc, :], n0[:, h, kc, :], EQb[:, iL:iL + 1], psN[:, 0:128], ALU.mult, ALU.add)
                    S.copy("act", n0b[:, h, kc, :], n0[:, h, kc, :])
                yield
            if st is G.lat:
                S.dma("pool", fm((st.H0, st.H1)[d], r0, r1, *sl), ht[:])


def scan1_both(G):

    G.S.push()
    interleave([scan1_gen(G, 0, 0), scan1_gen(G, 1, 0), scan1_gen(G, 0, 2), scan1_gen(G, 1, 2)])
    G.S.pop()


def build_all(TL, TC, upto=99):
    G = build(TL, TC)
    S = G.S
    steps = [
        lambda: G.ada(0), lambda: in0_phase(G), lambda: scan0_both(G),
        lambda: out_phase(G, 0), lambda: ffn2_phase(G, 0),
        lambda: G.ada(1), lambda: in1_phase(G), lambda: scan1_both(G),
        lambda: out_phase(G, 1), lambda: ffn2_phase(G, 1),
    ]
    for i, f in enumerate(steps):
        if i >= upto:
            break
        f()
    S.wait_all("pool")
    S.wait_all("sp")
    S.es.close()
    return G


def colvec(v):
    v = np.asarray(v, np.float32).reshape(-1)
    assert v.size % 128 == 0
    return v.reshape(-1, 128).T


def pack_shared(inp):
    vlay, NV = vec_layout()
    vecs = np.zeros((128, NV), np.float32)

    def put(name, v):
        o, n = vlay[name]
        cv = colvec(v)
        assert cv.shape[1] == n, (name, cv.shape, n)
        vecs[:, o:o + n] = cv
    for l in range(2):
        put(f"nmp{l}", inp["norm_mix_pre"][l])
        put(f"nmo{l}", inp["norm_mix_post"][l])
        put(f"nfp{l}", inp["norm_ffn_pre"][l])
        put(f"nfo{l}", inp["norm_ffn_post"][l])
        put(f"adab{l}", inp["ada_b"][l])
        for i in range(9):
            put(f"fconv{l}_{i}", inp["ffn_conv"][l].reshape(9, DFF)[i])
        put(f"fcb{l}", inp["ffn_conv_b"][l])
    put("agv", inp["a_gv"][0])
    for i in range(3):
        put(f"bshift{i}", inp["b_shift"][0][i])
    put("bw0", inp["b_w0"][0])
    put("ba0", inp["b_a0"][0])
    put("bkk", inp["b_kk"][0])
    put("bka", inp["b_ka"][0])
    put("brk", inp["b_rk"][0])
    put("blng", inp["b_ln_g"][0])
    put("blnb", inp["b_ln_b"][0])
    for i in range(3):
        put(f"cconv{i}", inp["c_conv"][0][i])
    put("ccb", inp["c_conv_b"][0])
    put("cskip", inp["c_skip"][0])
    put("cnorm", inp["c_norm"][0])
    o, _ = vlay["cgb"]
    vecs[0:8, o] = np.asarray(inp["c_bi"][0]).reshape(-1)
    vecs[8:16, o] = np.asarray(inp["c_bf"][0]).reshape(-1)
    sh = {"vecs": vecs, "consts": make_consts()}
    f32 = lambda a: np.ascontiguousarray(np.asarray(a, np.float32))
    sh["ada_w"] = f32(inp["ada_w"])
    sh["ffn_up"] = f32(inp["ffn_up"])
    sh["ffn_down"] = f32(inp["ffn_down"])
    sh["ab_w_in"] = f32(inp["ab_w_in"][0])
    sh["ab_w_out"] = f32(inp["ab_w_out"][0])
    sh["c_w_in"] = f32(inp["c_w_in"][0])
    sh["c_w_out"] = f32(inp["c_w_out"][0])
    sh["a_wsT"] = f32(np.transpose(np.asarray(inp["a_ws"][0]), (2, 0, 1)))
    sh["a_bs_bc"] = f32(np.broadcast_to(np.asarray(inp["a_bs"][0]).reshape(1, 512), (128, 512)))
    bup = np.zeros((128, 5, 512), np.float32)
    wu_, au_ = np.asarray(inp["b_w_up"][0]), np.asarray(inp["b_a_up"][0])
    for d_ in range(2):
        bup[d_ * 64:(d_ + 1) * 64, d_, :] = wu_[d_]
        bup[d_ * 64:(d_ + 1) * 64, 2 + d_, :] = au_[d_]
    bup[:, 4, :] = np.asarray(inp["b_g_up"][0])
    sh["b_up"] = bup
    bd = np.zeros((128, 3, KD, 128), np.float32)
    for wi, nm in enumerate(("c_wq", "c_wk", "c_wv")):
        w = np.asarray(inp[nm][0])
        for g in range(256):
            j, gl = divmod(g, 32)
            bd[gl * 4:gl * 4 + 4, wi, j, gl * 4:gl * 4 + 4] = w[g]
    sh["c_bd"] = bd
    return sh


def pack_core(inp, b, sh):
    m = dict(sh)
    m["xT"] = np.ascontiguousarray(np.asarray(inp["x"][b], np.float32).T)
    m["cxT"] = np.ascontiguousarray(np.asarray(inp["ctx"][b], np.float32).T)
    cv = np.stack([np.asarray(inp["c"][b], np.float32), np.asarray(inp["c_ctx"], np.float32)], axis=-1)
    m["cvec"] = np.ascontiguousarray(cv.reshape(KD, 128, 2).transpose(1, 0, 2))
    return m


TL_FULL = 8192
TC_FULL = 256
_CACHE = {}


def kernel(**inputs):
    inp = {k: np.asarray(v) for k, v in inputs.items()}
    B = inp["x"].shape[0]
    if "G" not in _CACHE:
        _CACHE["G"] = build_all(TL_FULL, TC_FULL)
    G = _CACHE["G"]
    sh = pack_shared(inp)
    ncores = 8
    in_maps = [pack_core(inp, c % B, sh) for c in range(ncores)]
    res = run_bass_kernel_spmd(G.nc, in_maps, core_ids=list(range(ncores)))
    out = np.stack([np.ascontiguousarray(res.results[b]["outT"].T) for b in range(B)], axis=0)
    return out.astype(np.float32)
```

```python
from contextlib import ExitStack
import concourse.bass as bass
import concourse.mybir as mybir

EPOCH = 8192
NDMA = 12


class V:
    __slots__ = ("ap", "keys")

    def __init__(self, ap, keys):
        self.ap = ap
        assert isinstance(keys, tuple) and all(isinstance(k, tuple) for k in keys), keys
        self.keys = keys


class Buf:
    def __init__(self, t, name):
        self.t = t
        self.name = name

    def __getitem__(self, idx):
        if isinstance(idx, tuple) and len(idx) >= 2 and isinstance(idx[1], int) and len(self.t.shape) >= 3:
            return V(self.t[idx], ((self.name, idx[1]),))
        return V(self.t[idx], ((self.name,),))

    def sub(self, sub, idx):
        return V(self.t[idx], ((self.name, sub),))

    def v(self, ap, sub=None):
        return V(ap, (((self.name,),) if sub is None else ((self.name, sub),)))


class Sched:
    def __init__(self, nc, same_engine_sync=True):
        self.nc = nc
        self.es = ExitStack()
        self.engs = {"pe": nc.tensor, "dve": nc.vector, "act": nc.scalar, "pool": nc.gpsimd, "sp": nc.sync}
        self.count = {e: 0 for e in self.engs}
        self.sems = {}
        self.waited = {e: {f: 0 for f in self.engs} for e in self.engs}
        self.dma_sems = {}
        self.dma_count = {e: 0 for e in self.engs}
        self.dma_waited = {e: {} for e in self.engs}
        self.state = {}
        self.same = same_engine_sync
        self.nwaits = 0
        self.stacks = [self.es]

    def push(self):
        self.stacks.append(ExitStack())

    def pop(self):
        self.barrier()
        self.stacks.pop().close()

    def sbuf(self, name, shape, dt):
        self.uid = getattr(self, "uid", 0) + 1
        name = f"{name}_{self.uid}"
        t = self.stacks[-1].enter_context(self.nc.sbuf_tensor(name, list(shape), dt))
        return Buf(t, name)

    def psum(self, name, shape, dt):
        t = self.stacks[-1].enter_context(self.nc.psum_tensor(name, list(shape), dt))
        return Buf(t, name)

    def _sem(self, e, ep):
        k = (e, ep)
        if k not in self.sems:
            self.sems[k] = self.es.enter_context(self.nc.semaphore(f"s_{e}_{ep}"))
        return self.sems[k]

    def _dsem(self, e, i):
        k = (e, i)
        if k not in self.dma_sems:
            self.dma_sems[k] = self.es.enter_context(self.nc.semaphore(f"d_{e}_{i}"))
        return self.dma_sems[k]

    def _entries(self, key):
        name = key[0]
        d = self.state.setdefault(name, {})
        if len(key) == 1:
            if None not in d:
                d[None] = [None, []]
            return list(d.values())
        sub = key[1]
        if sub not in d:
            d[sub] = [None, []]
        out = [d[sub]]
        if None in d:
            out.append(d[None])
        return out

    def _collect(self, reads, writes):
        deps = []
        for v in reads:
            for key in v.keys:
                for ent in self._entries(key):
                    if ent[0] is not None:
                        deps.append(ent[0])
        for v in writes:
            for key in v.keys:
                for ent in self._entries(key):
                    if ent[0] is not None:
                        deps.append(ent[0])
                    deps.extend(ent[1])
        return deps

    def _emit_waits(self, e, deps):
        eng = self.engs[e]
        need = {}
        for d in deps:
            if d[0] == "eng":
                _, f, n = d
                if f == e and (not self.same or e == "pe"):
                    continue
                if self.waited[e][f] >= n:
                    continue
                need[("eng", f)] = max(need.get(("eng", f), 0), n)
            else:
                _, q, i, val = d
                if self.dma_waited[e].get((q, i), 0) >= val:
                    continue
                need[("dma", q, i)] = max(need.get(("dma", q, i), 0), val)
        for k, n in need.items():
            if k[0] == "eng":
                f = k[1]
                ep = (n - 1) // EPOCH
                eng.wait_ge(self._sem(f, ep), n - ep * EPOCH)
                self.waited[e][f] = n
            else:
                _, q, i = k
                eng.wait_ge(self._dsem(q, i), n)
                self.dma_waited[e][(q, i)] = n
            self.nwaits += 1

    def _record(self, dep, reads, writes):
        for v in reads:
            for key in v.keys:
                ents = self._entries(key)
                if len(key) == 1:
                    for ent in ents:
                        ent[1].append(dep)
                else:
                    ents[0][1].append(dep)
        for v in writes:
            for key in v.keys:
                name = key[0]
                if len(key) == 1:
                    self.state[name] = {None: [dep, []]}
                else:
                    d = self.state[name]
                    d[key[1]] = [dep, []]
                    if None in d:
                        pass

    def op(self, e, fn, reads=(), writes=()):
        deps = self._collect(reads, writes)
        self._emit_waits(e, deps)
        n = self.count[e] + 1
        ep = (n - 1) // EPOCH
        inst = fn()
        inst.then_inc(self._sem(e, ep), 1)
        self.count[e] = n
        self._record(("eng", e, n), reads, writes)
        return inst

    def dma(self, q, out, in_, **kw):
        eng = self.engs[q]
        deps = self._collect([in_], [out])
        i = self.dma_count[q] % NDMA
        use = self.dma_count[q] // NDMA
        if use > 0:
            deps.append(("dma", q, i, 16 * use))
        self._emit_waits(q, deps)
        inst = eng.dma_start(out=out.ap, in_=in_.ap, **kw)
        inst.then_inc(self._dsem(q, i), 16)
        self.dma_count[q] += 1
        self._record(("dma", q, i, 16 * (use + 1)), [in_], [out])
        return inst

    def barrier(self):
        deps = [("eng", f, self.count[f]) for f in self.engs if self.count[f] > 0]
        for q in self.engs:
            c = self.dma_count[q]
            for i in range(min(c, NDMA)):
                uses = (c - 1 - i) // NDMA + 1
                deps.append(("dma", q, i, 16 * uses))
        for e in self.engs:
            self._emit_waits(e, deps)
        self.state = {}

    def wait_all(self, e):
        deps = [("eng", f, self.count[f]) for f in self.engs if self.count[f] > 0 and f != e]
        for q in self.engs:
            c = self.dma_count[q]
            for i in range(min(c, NDMA)):
                uses = (c - 1 - i) // NDMA + 1
                deps.append(("dma", q, i, 16 * uses))
        self._emit_waits(e, deps)

    def matmul(self, out, lhsT, rhs, start=True, stop=True, **kw):
        return self.op("pe", lambda: self.nc.tensor.matmul(out.ap, lhsT.ap, rhs.ap, start=start, stop=stop, **kw),
                       reads=[lhsT, rhs] + ([] if start else [out]), writes=[out])

    def transpose(self, out, in_, ident):
        return self.op("pe", lambda: self.nc.tensor.transpose(out.ap, in_.ap, ident.ap), reads=[in_, ident], writes=[out])

    def act(self, out, in_, func, bias=None, scale=None, accum_out=None, eng="act"):
        kw = {}
        reads = [in_]
        writes = [out]
        if bias is not None:
            if isinstance(bias, V):
                kw["bias"] = bias.ap
                reads.append(bias)
            else:
                kw["bias"] = bias
        if scale is not None:
            if isinstance(scale, V):
                kw["scale"] = scale.ap
                reads.append(scale)
            else:
                kw["scale"] = scale
        if accum_out is not None:
            kw["accum_out"] = accum_out.ap
            writes.append(accum_out)
        return self.op("act", lambda: self.nc.scalar.activation(out.ap, in_.ap, func, **kw), reads=reads, writes=writes)

    def tt(self, e, out, in0, in1, op):
        return self.op(e, lambda: self.engs[e].tensor_tensor(out.ap, in0.ap, in1.ap, op), reads=[in0, in1], writes=[out])

    def ts(self, e, out, in0, s1, op0, s2=None, op1=None):
        reads = [in0]
        a1 = s1
        a2 = s2
        if isinstance(s1, V):
            reads.append(s1)
            a1 = s1.ap
        if isinstance(s2, V):
            reads.append(s2)
            a2 = s2.ap
        if op1 is None:
            return self.op(e, lambda: self.engs[e].tensor_scalar(out.ap, in0.ap, a1, None, op0), reads=reads, writes=[out])
        return self.op(e, lambda: self.engs[e].tensor_scalar(out.ap, in0.ap, a1, a2, op0, op1), reads=reads, writes=[out])

    def stt(self, out, in0, s, in1, op0, op1, e="dve"):
        reads = [in0, in1]
        a = s
        if isinstance(s, V):
            reads.append(s)
            a = s.ap
        return self.op(e, lambda: self.engs[e].scalar_tensor_tensor(out.ap, in0.ap, a, in1.ap, op0, op1), reads=reads, writes=[out])

    def copy(self, e, out, in_):
        if e == "act":
            return self.op(e, lambda: self.nc.scalar.copy(out.ap, in_.ap), reads=[in_], writes=[out])
        return self.op(e, lambda: self.engs[e].tensor_copy(out.ap, in_.ap), reads=[in_], writes=[out])

    def memset(self, e, out, val):
        return self.op(e, lambda: self.engs[e].memset(out.ap, val), reads=[], writes=[out])

import numpy as np
import concourse.bass as bass
import concourse.mybir as mybir
from concourse.bass_utils import run_bass_kernel_spmd

F32 = mybir.dt.float32
BF16 = mybir.dt.bfloat16
AF = mybir.ActivationFunctionType
ALU = mybir.AluOpType

D = 1024
KD = 8
DFF = 2816
NF = 22
EPS = 1e-6
LN_EPS = 64e-5
TS = 256
LR = 64
LM = 128


def vec_layout():
    lay = {}
    off = 0

    def add(name, n):
        nonlocal off
        lay[name] = (off, n)
        off += n
    for l in range(2):
        for nm in ("nmp", "nmo", "nfp", "nfo"):
            add(f"{nm}{l}", 8)
        add(f"adab{l}", 48)
        for i in range(9):
            add(f"fconv{l}_{i}", NF)
        add(f"fcb{l}", NF)
    add("agv", 4)
    for i in range(3):
        add(f"bshift{i}", 12)
    add("bw0", 8)
    add("ba0", 8)
    add("bkk", 4)
    add("bka", 4)
    add("brk", 4)
    add("blng", 4)
    add("blnb", 4)
    for i in range(3):
        add(f"cconv{i}", 8)
    add("ccb", 8)
    add("cskip", 8)
    add("cnorm", 8)
    add("cgb", 1)
    return lay, off


def const_layout():
    lay = {}
    off = 0
    for name, n in (("ident", 128), ("ones", 128), ("blk64", 128), ("su", 512), ("sl", 512), ("iu", 512), ("il", 512),
                    ("id8", 512), ("mu", 128), ("ml", 128), ("sel", 2048), ("seg", TS)):
        lay[name] = (off, n)
        off += n
    return lay, off


def make_consts():
    lay, n = const_layout()
    c = np.zeros((128, n), np.float32)
    c[:, lay["ident"][0]:lay["ident"][0] + 128] = np.eye(128)
    c[:, lay["ones"][0]:lay["ones"][0] + 128] = 1.0
    b = np.zeros((128, 128), np.float32)
    b[:64, :64] = 1
    b[64:, 64:] = 1
    c[:, lay["blk64"][0]:lay["blk64"][0] + 128] = b
    su = np.triu(np.ones((64, 64), np.float32), 1)
    sl = np.tril(np.ones((64, 64), np.float32), -1)
    for nm, m in (("su", su), ("sl", sl), ("iu", su + np.eye(64)), ("il", sl + np.eye(64)), ("id8", np.eye(64))):
        c[:64, lay[nm][0]:lay[nm][0] + 512] = np.tile(m, (1, 8))
    c[:, lay["mu"][0]:lay["mu"][0] + 128] = np.triu(np.ones((128, 128), np.float32))
    c[:, lay["ml"][0]:lay["ml"][0] + 128] = np.tril(np.ones((128, 128), np.float32))
    for r in range(16):
        c[r, lay["sel"][0] + r * 128: lay["sel"][0] + (r + 1) * 128] = 1.0
    seg = np.ones((TS,), np.float32)
    seg[::LR] = 0.0
    c[:, lay["seg"][0]:lay["seg"][0] + TS] = seg[None, :]
    return c


class Ctx:
    pass


def build(TL, TC, nlayers=2):

    nc = bass.Bass("TRN2", target_bir_lowering=False)
    S = Sched(nc)
    vlay, NV = vec_layout()
    clay, NCON = const_layout()

    def dram(name, shape, dt=F32, kind="Internal"):
        return Buf(nc.dram_tensor(name, list(shape), dt, kind=kind), name)

    EI = "ExternalInput"
    xT = dram("xT", [D, TL], kind=EI)
    cxT = dram("cxT", [D, TC], kind=EI)
    cvec = dram("cvec", [128, KD, 2], kind=EI)
    vecs_d = dram("vecs", [128, NV], kind=EI)
    consts_d = dram("consts", [128, NCON], kind=EI)
    ada_w = dram("ada_w", [2, D, 6 * D], kind=EI)
    ffn_up = dram("ffn_up", [2, D, 2 * DFF], kind=EI)
    ffn_down = dram("ffn_down", [2, DFF, D], kind=EI)
    ab_w_in = dram("ab_w_in", [D, 2944], kind=EI)
    ab_w_out = dram("ab_w_out", [D, D], kind=EI)
    c_w_in = dram("c_w_in", [D, 2064], kind=EI)
    c_w_out = dram("c_w_out", [D, D], kind=EI)
    a_wsT = dram("a_wsT", [128, 4, 128], kind=EI)
    a_bs_bc = dram("a_bs_bc", [128, 512], kind=EI)
    b_up = dram("b_up", [128, 5, 512], kind=EI)
    c_bd = dram("c_bd", [128, 3, KD, 128], kind=EI)
    outT = dram("outT", [D, TL], kind="ExternalOutput")

    class Stream:
        pass
    lat = Stream(); lat.T = TL; lat.name = "l"; lat.idx = 0
    ctx = Stream(); ctx.T = TC; ctx.name = "c"; ctx.idx = 1
    lat.res = [xT, dram("lR1", [D, TL]), dram("lR2", [D, TL]), dram("lR3", [D, TL]), outT]
    ctx.res = [cxT, dram("cR1", [D, TC]), dram("cR2", [D, TC]), None, None]
    for st in (lat, ctx):
        T = st.T
        n = st.name
        st.AO = dram(n + "AO", [512, T], BF16)
        for nm, rows in (("R", 512), ("KD0", 512), ("KD1", 512), ("VV", 512), ("KK", 512), ("B0", 512), ("B1", 512),
                         ("LD0", 512), ("LD1", 512), ("GG", 512), ("BV", 512), ("Y0", 512), ("Y1", 512),
                         ("FB", DFF),
                         ("MQ", D), ("MK", D), ("XCV", D), ("SZ", D), ("H0", D), ("H1", D)):
            setattr(st, nm, dram(n + nm, [rows, T]))
        st.FA = dram(n + "FA", [DFF, T], BF16)
        st.KTOK = dram(n + "KTOK", [T, D])
        st.VTOK = dram(n + "VTOK", [T, D])
        st.GATE = dram(n + "GATE", [16, T])

    def fm(buf, r0, r1, c0, c1):
        return buf.v(buf.t[r0:r1, c0:c1].rearrange("(j p) t -> p j t", p=128))

    vecs = S.sbuf("vecs_s", [128, NV], F32)
    con = S.sbuf("con_s", [128, NCON], F32)
    mod = S.sbuf("mod_s", [128, 48, 2], F32)
    S.dma("sp", vecs[:], vecs_d[:])
    S.dma("sp", con[:], consts_d[:])

    def vc(name, j=0, n=1):
        o = vlay[name][0] + j
        return vecs[:, o:o + n]

    def vcr(name, rows):
        o = vlay[name][0]
        return vecs[0:rows, o:o + 1]

    def cc(name, rows=128, c0=0, n=None):
        o, w = clay[name]
        if n is None:
            n = w
        return con[0:rows, o + c0:o + c0 + n]

    ones_b = S.sbuf("ones_b", [128, 128], BF16)
    S.memset("dve", ones_b[:], 1.0)
    banks = [S.psum(f"psb{i}", [128, 512], F32) for i in range(8)]
    bank_i = [0]

    def nb():
        b = banks[bank_i[0] % 8]
        bank_i[0] += 1
        return b

    rr = [0]

    def ew():
        rr[0] += 1
        return "dve" if rr[0] % 2 else "pool"

    def ada(l):
        S.push()
        cs = S.sbuf("ada_c", [128, KD, 2], F32)
        sc = S.sbuf("ada_sc", [128, KD, 2], F32)
        S.dma("sp", cs[:], cvec[:])
        S.act(sc[:], cs[:], AF.Silu)
        wt = [S.sbuf(f"ada_w{i}", [128, KD, 768], F32) for i in range(2)]
        for g in range(8):
            w = wt[g % 2]
            S.dma("sp", w[:], ada_w.v(ada_w.t[l, :, g * 768:(g + 1) * 768].rearrange("(j p) f -> p j f", p=128)))
            for jj in range(6):
                col = g * 6 + jj
                ps = nb()
                for k in range(KD):
                    S.matmul(ps[:, 0:2], w[:, k, jj * 128:(jj + 1) * 128], sc[:, k, :], start=(k == 0), stop=(k == KD - 1))
                S.ts("dve", mod[:, col, :], ps[:, 0:2], vc(f"adab{l}", col), ALU.add)
        S.pop()

    def load_w_bf16(dst, src_ap_fn, ncols, stage):
        i = 0
        c0 = 0
        while c0 < ncols:
            c1 = min(c0 + stage[0].t.shape[2], ncols)
            st = stage[i % len(stage)]
            S.dma("sp", st.v(st.t[:, :, 0:c1 - c0]), src_ap_fn(c0, c1))
            S.copy(ew(), dst.v(dst.t[:, :, c0:c1]), st.v(st.t[:, :, 0:c1 - c0]))
            c0 = c1
            i += 1

    def rstd_bcast(sq_chunks, n, out_rstd, ones_v, scale, eps, tmp):
        ps = nb()
        for j, sqv in enumerate(sq_chunks):
            S.matmul(ps[:, 0:n], ones_v, sqv, start=(j == 0), stop=(j == len(sq_chunks) - 1))
        S.ts("dve", tmp, ps[:, 0:n], scale, ALU.mult, eps, ALU.add)
        S.act(tmp, tmp, AF.Sqrt)
        S.op("dve", lambda: nc.vector.reciprocal(out_rstd.ap, tmp.ap), reads=[tmp], writes=[out_rstd])

    def modcol(i, j, st):
        return mod[:, i * 8 + j, st.idx:st.idx + 1]

    def make_coef(l, st, gname, scale_i, out):
        sv = mod[:, scale_i * 8:(scale_i + 1) * 8, st.idx]
        S.tt("dve", out, sv, vc(gname, 0, 8), ALU.mult)
        S.tt("dve", out, out, vc(gname, 0, 8), ALU.add)

    def make_gcoef(l, st, gname, gate_i, out):
        sv = mod[:, gate_i * 8:(gate_i + 1) * 8, st.idx]
        S.tt("dve", out, sv, vc(gname, 0, 8), ALU.mult)

    def norm_mod(x, n, coef, shift_i, st, hout, sq, rstd, tmp):
        for j in range(KD):
            S.act(hout[:, j, 0:n], x[:, j, 0:n], AF.Square)
        rstd_bcast([hout[:, j, 0:n] for j in range(KD)], n, rstd.v(rstd.t[:, 0:n]), ones_b[:], 1.0 / D, EPS, tmp.v(tmp.t[:, 0:n]))
        for j in range(KD):
            S.stt(sq[:, j, 0:n], x[:, j, 0:n], coef.v(coef.t[:, j:j + 1]), rstd.v(rstd.t[:, 0:n]), ALU.mult, ALU.mult)
            S.act(hout[:, j, 0:n], sq[:, j, 0:n], AF.Identity, bias=modcol(shift_i, j, st), scale=1.0)

    def post_norm_res(y, n, gcoef, xin, xout, sq, rstd, tmp, sqb=None):
        if sqb is None:
            S.act(sq.v(sq.t[:, :, 0:n]), y.v(y.t[:, :, 0:n]), AF.Square)
            rstd_bcast([sq.v(sq.t[:, j, 0:n]) for j in range(KD)], n, rstd.v(rstd.t[:, 0:n]), cc("ones"), 1.0 / D, EPS, tmp.v(tmp.t[:, 0:n]))
        else:
            for j in range(KD):
                S.act(sqb[:, j, 0:n], y[:, j, 0:n], AF.Square)
            rstd_bcast([sqb[:, j, 0:n] for j in range(KD)], n, rstd.v(rstd.t[:, 0:n]), ones_b[:], 1.0 / D, EPS, tmp.v(tmp.t[:, 0:n]))
        for j in range(KD):
            S.stt(sq[:, j, 0:n], y[:, j, 0:n], gcoef.v(gcoef.t[:, j:j + 1]), rstd.v(rstd.t[:, 0:n]), ALU.mult, ALU.mult)
            S.tt("dve" if j % 2 else "pool", xout[:, j, 0:n], sq[:, j, 0:n], xin[:, j, 0:n], ALU.add)

    def load_halo(dst, src, t0, n, T, rows0, rows1):
        lo, hi = t0 - 1, t0 + n + 1
        clo, chi = max(lo, 0), min(hi, T)
        if lo < 0:
            S.memset("dve", dst.v(dst.t[:, :, 0:1]), 0.0)
        if hi > T:
            S.memset("dve", dst.v(dst.t[:, :, n + 1:n + 2]), 0.0)
        S.dma("sp", dst.v(dst.t[:, :, clo - lo:chi - lo]), fm(src, rows0, rows1, clo, chi))

    def tiles(T):
        return [(t0, min(TS, T - t0)) for t0 in range(0, T, TS)]

    G = Ctx()
    G.nc, G.S, G.lat, G.ctx = nc, S, lat, ctx
    G.fm, G.vc, G.cc, G.nb, G.ew, G.mod, G.vcr = fm, vc, cc, nb, ew, mod, vcr
    G.ada, G.load_w_bf16, G.rstd_bcast, G.modcol = ada, load_w_bf16, rstd_bcast, modcol
    G.make_coef, G.make_gcoef, G.norm_mod, G.post_norm_res, G.load_halo, G.tiles = make_coef, make_gcoef, norm_mod, post_norm_res, load_halo, tiles
    G.w = dict(ffn_up=ffn_up, ffn_down=ffn_down, ab_w_in=ab_w_in, ab_w_out=ab_w_out, c_w_in=c_w_in, c_w_out=c_w_out,
               a_wsT=a_wsT, a_bs_bc=a_bs_bc, b_up=b_up, c_bd=c_bd)
    G.banks = banks
    return G


def in0_phase(G):
    S, nc, fm, vc, cc, nb, ew = G.S, G.nc, G.fm, G.vc, G.cc, G.nb, G.ew
    W = G.w
    S.push()
    NH = TS + 2
    wb = S.sbuf("i0_wb", [128, KD, 2944], BF16)
    stage = [S.sbuf(f"i0_st{i}", [128, KD, 256], F32) for i in range(2)]
    G.load_w_bf16(wb, lambda c0, c1: W["ab_w_in"].v(W["ab_w_in"].t[:, c0:c1].rearrange("(j p) f -> p j f", p=128)), 2944, stage)
    wsT = S.sbuf("i0_wsT", [128, 4, 128], BF16)
    S.dma("sp", stage[0].v(stage[0].t[:, 0:4, 0:128]), W["a_wsT"][:])
    S.copy("dve", wsT[:], stage[0].v(stage[0].t[:, 0:4, 0:128]))
    bsb = S.sbuf("i0_bsb", [128, 512], F32)
    S.dma("sp", bsb[:], W["a_bs_bc"][:])
    upf = S.sbuf("i0_upf", [128, 5, 512], F32)
    upb = S.sbuf("i0_upb", [128, 5, 512], BF16)
    S.dma("sp", upf[:], W["b_up"][:])
    S.copy("dve", upb[:], upf[:])
    omka = S.sbuf("i0_omka", [128, 4], F32)
    S.ts("dve", omka[:], vc("bka", 0, 4), -1.0, ALU.mult, 1.0, ALU.add)
    coef = S.sbuf("i0_coef", [128, 8], F32)
    xt = S.sbuf("i0_xt", [128, KD, NH], F32)
    sq = S.sbuf("i0_sq", [128, KD, NH], F32)
    rstd = S.sbuf("i0_rstd", [128, NH], F32)
    tmp = S.sbuf("i0_tmp", [128, NH], F32)
    h = S.sbuf("i0_h", [128, KD, NH], BF16)
    ug_2 = [S.sbuf("i0_ug%d" % i, [128, TS], F32) for i in range(2)]
    vg_2 = [S.sbuf("i0_vg%d" % i, [128, TS], F32) for i in range(2)]
    vsq_2 = [S.sbuf("i0_vsq%d" % i, [128, TS], F32) for i in range(2)]
    vr_2 = [S.sbuf("i0_vr%d" % i, [128, TS], F32) for i in range(2)]
    vt2_2 = [S.sbuf("i0_vt2%d" % i, [128, TS], F32) for i in range(2)]
    vtok_2 = [S.sbuf("i0_vtok%d" % i, [128, 128], BF16) for i in range(2)]
    zt_2 = [S.sbuf("i0_zt%d" % i, [128, 128], F32) for i in range(2)]
    ao = S.sbuf("i0_ao", [128, 4, TS], BF16)
    rkv = S.sbuf("i0_rkv", [128, 12, TS], F32)
    ct = S.sbuf("i0_ct", [128, TS], F32)
    lor = S.sbuf("i0_lor", [128, 3, TS], BF16)
    LDt = S.sbuf("i0_LD", [128, 8, TS], F32)
    At = S.sbuf("i0_A", [128, 8, TS], F32)
    Gt = S.sbuf("i0_G", [128, 4, TS], F32)
    kk = S.sbuf("i0_kk", [128, 4, TS], F32)
    kd = S.sbuf("i0_kd", [128, 2, 4, TS], F32)
    bb = S.sbuf("i0_bb", [128, 2, 4, TS], F32)
    rk = S.sbuf("i0_rk", [128, 2, 4, TS], F32)
    bv = S.sbuf("i0_bv", [128, 4, TS], F32)

    for st in (G.ctx, G.lat):
        T = st.T
        G.make_coef(0, st, "nmp0", 1, coef[:])
        for (t0, n) in G.tiles(T):
            nh = n + 2
            G.load_halo(xt, st.res[0], t0, n, T, 0, D)
            G.norm_mod(xt, nh, coef, 0, st, h, sq, rstd, tmp)

            def proj(oc, msz=128):
                ps = nb()
                for k in range(KD):
                    S.matmul(ps[0:msz, 0:nh], wb[:, k, oc * 128:oc * 128 + msz], h[:, k, 0:nh], start=(k == 0), stop=(k == KD - 1))
                return ps
            for g in range(4):
                ug, vg, vsq, vr, vt2 = ug_2[g % 2], vg_2[g % 2], vsq_2[g % 2], vr_2[g % 2], vt2_2[g % 2]
                psu = proj(g)
                S.act(ug[:, 0:n], psu[:, 1:n + 1], AF.Gelu_apprx_tanh)
                psv = proj(4 + g)
                S.act(vg[:, 0:n], psv[:, 1:n + 1], AF.Gelu_apprx_tanh)
                S.act(vsq[:, 0:n], vg[:, 0:n], AF.Square)
                G.rstd_bcast([vsq[:, 0:n]], n, vr[:, 0:n], cc("ones"), 1.0 / 128, EPS, vt2[:, 0:n])
                S.stt(vsq[:, 0:n], vg[:, 0:n], vc("agv", g), vr[:, 0:n], ALU.mult, ALU.mult)
                for c0 in range(0, n, 128):
                    vtok, zt = vtok_2[(c0 // 128) % 2], zt_2[(c0 // 128) % 2]
                    pt = nb()
                    S.transpose(pt[:, 0:128], vsq[:, c0:c0 + 128], cc("ident"))
                    S.copy("act", vtok[:], pt[:, 0:128])
                    pz = nb()
                    S.matmul(pz[:, 0:128], vtok[:], wsT[:, g, :])
                    S.tt("dve", zt[:], pz[:, 0:128], bsb[:, g * 128:(g + 1) * 128], ALU.add)
                    S.tt("pool", ao[:, g, c0:c0 + 128], zt[:], ug[:, c0:c0 + 128], ALU.mult)
            S.dma("pool", fm(st.AO, 0, 512, t0, t0 + n), ao[:, :, 0:n])
            lo0 = 1 if t0 == 0 else 0
            hi2 = n - 1 if t0 + n >= T else n
            for i in range(12):
                ps = proj(8 + i)
                S.ts("dve", ct[:, 0:n], ps[:, 1:n + 1], vc("bshift1", i), ALU.mult)
                S.stt(ct[:, lo0:n], ps[:, lo0:n], vc("bshift0", i), ct[:, lo0:n], ALU.mult, ALU.add)
                S.stt(rkv[:, i, 0:hi2], ps[:, 2:hi2 + 2], vc("bshift2", i), ct[:, 0:hi2], ALU.mult, ALU.add)
                if hi2 < n:
                    S.copy("dve", rkv[:, i, hi2:n], ct[:, hi2:n])
            ps = proj(20)
            S.act(lor[:, 0, 0:n], ps[:, 1:n + 1], AF.Tanh)
            ps = proj(21)
            S.copy("act", lor[:, 1, 0:n], ps[:, 1:n + 1])
            ps = proj(22)
            S.act(lor[:, 2, 0:n], ps[:, 1:n + 1], AF.Sigmoid)
            for d in range(2):
                for c in range(4):
                    ps = nb()
                    S.matmul(ps[:, 0:n], upb[:, d, c * 128:(c + 1) * 128], lor[:, 0, 0:n])
                    S.act(LDt[:, d * 4 + c, 0:n], ps[:, 0:n], AF.Sigmoid, bias=vc("bw0", d * 4 + c), scale=1.0)
                    ps = nb()
                    S.matmul(ps[:, 0:n], upb[:, 2 + d, c * 128:(c + 1) * 128], lor[:, 1, 0:n])
                    S.act(At[:, d * 4 + c, 0:n], ps[:, 0:n], AF.Sigmoid, bias=vc("ba0", d * 4 + c), scale=1.0)
            S.ts("pool", LDt[:, :, 0:n], LDt[:, :, 0:n], -float(np.exp(-0.5)), ALU.mult)
            for c in range(4):
                ps = nb()
                S.matmul(ps[:, 0:n], upb[:, 4, c * 128:(c + 1) * 128], lor[:, 2, 0:n])
                S.copy("act", Gt[:, c, 0:n], ps[:, 0:n])
            for c in range(4):
                S.ts("dve", kk[:, c, 0:n], rkv[:, 4 + c, 0:n], vc("bkk", c), ALU.mult)
            S.act(sq[:, 0:4, 0:n], kk[:, :, 0:n], AF.Square)
            for c in range(4):
                ps = nb()
                S.matmul(ps[:, 0:n], cc("blk64"), sq[:, c, 0:n])
                S.ts("dve", ct[:, 0:n], ps[:, 0:n], 1e-12, ALU.max)
                S.act(ct[:, 0:n], ct[:, 0:n], AF.Sqrt)
                S.op("dve", lambda: nc.vector.reciprocal(ct.t[:, 0:n], ct.t[:, 0:n]), reads=[ct[:]], writes=[ct[:]])
                S.tt("dve", kk[:, c, 0:n], kk[:, c, 0:n], ct[:, 0:n], ALU.mult)
            for d in range(2):
                for c in range(4):
                    S.ts("pool", kd[:, d, c, 0:n], At[:, d * 4 + c, 0:n], vc("bka", c), ALU.mult, omka[:, c:c + 1], ALU.add)
                S.tt("dve", kd[:, d, :, 0:n], kd[:, d, :, 0:n], rkv[:, 4:8, 0:n], ALU.mult)
                S.tt("pool", bb[:, d, :, 0:n], kk[:, :, 0:n], At[:, d * 4:d * 4 + 4, 0:n], ALU.mult)
                S.tt("dve", rk[:, d, :, 0:n], kd[:, d, :, 0:n], rkv[:, 0:4, 0:n], ALU.mult)
                for c in range(4):
                    S.ts("pool", rk[:, d, c, 0:n], rk[:, d, c, 0:n], vc("brk", c), ALU.mult)
            for c in range(4):
                ps = nb()
                for d in range(2):
                    S.matmul(ps[:, 0:n], cc("blk64"), rk[:, d, c, 0:n], start=(d == 0), stop=(d == 1))
                S.tt("dve", bv[:, c, 0:n], ps[:, 0:n], rkv[:, 8 + c, 0:n], ALU.mult)
            sl = (t0, t0 + n)
            S.dma("pool", fm(st.R, 0, 512, *sl), rkv[:, 0:4, 0:n])
            S.dma("pool", fm(st.VV, 0, 512, *sl), rkv[:, 8:12, 0:n])
            S.dma("pool", fm(st.KK, 0, 512, *sl), kk[:, :, 0:n])
            S.dma("pool", fm(st.GG, 0, 512, *sl), Gt[:, :, 0:n])
            S.dma("pool", fm(st.BV, 0, 512, *sl), bv[:, :, 0:n])
            for d in range(2):
                S.dma("pool", fm((st.KD0, st.KD1)[d], 0, 512, *sl), kd[:, d, :, 0:n])
                S.dma("pool", fm((st.B0, st.B1)[d], 0, 512, *sl), bb[:, d, :, 0:n])
                S.dma("pool", fm((st.LD0, st.LD1)[d], 0, 512, *sl), LDt[:, d * 4:d * 4 + 4, 0:n])
    S.pop()


def scan0_gen(G, d):
    S, nc, fm, vc, cc, nb, ew = G.S, G.nc, G.fm, G.vc, G.cc, G.nb, G.ew
    NCH = TSS // LR
    f4 = lambda nm: S.sbuf(f"s0_{nm}", [128, 4, TSS], F32)
    Rr, KDd, VV, KKk, Bd, LD = f4("R"), f4("KD"), f4("VV"), f4("KK"), f4("B"), f4("LD")
    f4b = lambda nm: S.sbuf(f"s0_{nm}", [128, 4, TSS], BF16)
    lg, t1, kbar, bbar = f4("lg"), f4("t1"), f4("kbar"), f4("bbar")
    kkt, khat, bhat, rt = f4b("kkt"), f4b("khat"), f4b("bhat"), f4b("rt")
    gg = f4("g")
    yt = f4("y")
    fbd = lambda nm: S.sbuf(f"s0_{nm}", [128, 4, NCH, 128], BF16)
    kkt_bd, bhat_bd, rt_bd = fbd("kktbd"), fbd("bhatbd"), fbd("rtbd")
    vtok = S.sbuf("s0_vtok", [64, NCH, 512], BF16)
    kbtok = S.sbuf("s0_kbtok", [64, NCH, 512], BF16)
    bbtok = S.sbuf("s0_bbtok", [64, NCH, 512], BF16)
    Hbb = S.sbuf("s0_Hb", [128, 4, 128], BF16)
    Rmb = S.sbuf("s0_Rmb", [64, 512], BF16)
    Hbd = S.sbuf("s0_H", [128, 4, 128], F32)
    mk = lambda nm: S.sbuf(f"s0_{nm}", [64, 512], BF16)
    AkkT, ArkT, ArbT, Wsb, Un = [mk(x) for x in ("AkkT", "ArkT", "ArbT", "Wsb", "Un")]
    Pa, Pta, Pb, Ptb = [S.sbuf(f"s0_{x}", [64, 512], F32) for x in ("Pa", "Pta", "Pb", "Ptb")]
    Rm = S.sbuf("s0_Rm", [64, 512], F32)
    S.memset("dve", Hbd[:], 0.0)
    S.memset("dve", Hbb[:], 0.0)
    S.memset("pool", kkt_bd[:], 0.0)
    S.memset("pool", bhat_bd[:], 0.0)
    S.memset("pool", rt_bd[:], 0.0)
    strict = cc("su", 64) if d == 0 else cc("sl", 64)
    strictT = cc("sl", 64) if d == 0 else cc("su", 64)
    incl = cc("iu", 64) if d == 0 else cc("il", 64)
    id8 = cc("id8", 64)

    def blk(buf, h):
        return buf[0:64, h * 64:(h + 1) * 64]

    def mmd(v):
        if not INV_FP32R:
            return v

        return V(v.ap.bitcast(mybir.dt.float32r), v.keys)

    def to_bd(dst, src, nch, n):
        for hp in range(2):
            rows = slice(hp * 64, hp * 64 + 64)
            S.copy("pool" if hp else "dve", dst.v(dst.t[rows, :, 0:nch, hp * 64:hp * 64 + 64]),
                   src.v(src.t[rows, :, 0:n].rearrange("p c (k l) -> p c k l", l=LR)))

    for st in (G.ctx, G.lat):
        T = st.T
        tl = [(t0, min(TSS, T - t0)) for t0 in range(0, T, TSS)]
        if d == 1:
            tl = tl[::-1]
        for (t0, n) in tl:
            sl = (t0, t0 + n)
            for dst, src in ((Rr, st.R), (KDd, (st.KD0, st.KD1)[d]), (VV, st.VV), (KKk, st.KK), (Bd, (st.B0, st.B1)[d]), (LD, (st.LD0, st.LD1)[d])):
                S.dma("sp", dst[:, :, 0:n], fm(src, 0, 512, *sl))
            nch = n // LR
            for c in range(4):
                S.op("dve", lambda c=c: nc.vector.tensor_tensor_scan(lg.t[:, c, 0:n], cc("seg").ap[:, 0:n], LD.t[:, c, 0:n], 0.0, ALU.mult, ALU.add),
                     reads=[cc("seg"), LD[:]], writes=[lg[:]])
            lg4 = lambda b: b.t[:, :, 0:n].rearrange("p c (k l) -> p c k l", l=LR)
            if d == 1:
                tot = lg.v(lg4(lg)[:, :, :, LR - 1:LR].broadcast_to([128, 4, nch, LR]))
                S.tt("dve", t1.v(lg4(t1)), tot, lg.v(lg4(lg)), ALU.subtract)
                S.tt("dve", lg[:, :, 0:n], t1[:, :, 0:n], LD[:, :, 0:n], ALU.add)
            ie = (LR - 1) if d == 0 else 0
            S.act(gg[:, :, 0:n], lg[:, :, 0:n], AF.Exp)
            S.tt("pool", rt[:, :, 0:n], Rr[:, :, 0:n], gg[:, :, 0:n], ALU.mult)
            S.act(t1[:, :, 0:n], lg[:, :, 0:n], AF.Exp, scale=-1.0)
            S.tt("dve", khat[:, :, 0:n], KDd[:, :, 0:n], t1[:, :, 0:n], ALU.mult)
            S.tt("pool", bhat[:, :, 0:n], Bd[:, :, 0:n], t1[:, :, 0:n], ALU.mult)
            S.tt("dve", t1[:, :, 0:n], lg[:, :, 0:n], LD[:, :, 0:n], ALU.subtract)
            S.act(t1[:, :, 0:n], t1[:, :, 0:n], AF.Exp)
            S.tt("dve", kkt[:, :, 0:n], KKk[:, :, 0:n], t1[:, :, 0:n], ALU.mult)
            lgL = lg.v(lg4(lg)[:, :, :, ie:ie + 1].broadcast_to([128, 4, nch, LR]))
            S.tt("dve", t1.v(lg4(t1)), lgL, lg.v(lg4(lg)), ALU.subtract)
            S.act(t1[:, :, 0:n], t1[:, :, 0:n], AF.Exp)
            S.tt("dve", kbar[:, :, 0:n], KDd[:, :, 0:n], t1[:, :, 0:n], ALU.mult)
            S.tt("pool", bbar[:, :, 0:n], Bd[:, :, 0:n], t1[:, :, 0:n], ALU.mult)
            to_bd(kkt_bd, kkt, nch, n)
            to_bd(bhat_bd, bhat, nch, n)
            to_bd(rt_bd, rt, nch, n)
            for ch in range(nch):
                for src, dst in ((VV, vtok), (kbar, kbtok), (bbar, bbtok)):
                    ps = nb()
                    for c in range(4):
                        S.transpose(ps[0:64, c * 128:(c + 1) * 128], src[:, c, ch * LR:(ch + 1) * LR], cc("ident"))
                    S.copy("act", dst[:, ch, :], ps[0:64, :])
            chs = list(range(nch))
            if d == 1:
                chs = chs[::-1]
            for ch in chs:
                c0 = ch * LR
                pl = lambda buf, pr: buf[:, pr, c0:c0 + LR]
                bdv = lambda buf, pr: buf[:, pr, ch, :]
                pp = lambda ps, pr: ps[0:64, pr * 128:(pr + 1) * 128]
                psN, psNt = nb(), nb()
                for pr in range(4):
                    S.matmul(pp(psN, pr), pl(bhat, pr), bdv(kkt_bd, pr))
                    S.matmul(pp(psNt, pr), pl(kkt, pr), bdv(bhat_bd, pr))
                S.tt("dve", Pa[:], psN[0:64, :], strict, ALU.mult)
                S.tt("dve", Pta[:], psNt[0:64, :], strictT, ALU.mult)
                psKK, psRK, psRB = nb(), nb(), nb()
                for pr in range(4):
                    S.matmul(pp(psKK, pr), pl(khat, pr), bdv(kkt_bd, pr))
                    S.matmul(pp(psRK, pr), pl(khat, pr), bdv(rt_bd, pr))
                    S.matmul(pp(psRB, pr), pl(bhat, pr), bdv(rt_bd, pr))
                S.tt("dve", AkkT[:], psKK[0:64, :], strict, ALU.mult)
                S.tt("dve", ArkT[:], psRK[0:64, :], incl, ALU.mult)
                S.tt("dve", ArbT[:], psRB[0:64, :], incl, ALU.mult)
                S.tt("pool", Rm[:], id8, Pa[:], ALU.subtract)
                yield
                P, Pt, P2, Pt2 = Pa, Pta, Pb, Ptb
                for lev in range(5):
                    ps1, ps2 = nb(), nb()
                    for h in range(8):
                        if lev < 4:
                            S.matmul(blk(ps1, h), mmd(blk(Pt, h)), mmd(blk(P, h)))
                        S.matmul(blk(ps2, h), mmd(blk(P, h)), mmd(blk(Pt, h)))
                    if lev < 4:
                        S.copy("act", P2[:], ps1[0:64, :])
                    S.copy("dve", Pt2[:], ps2[0:64, :])
                    ps3 = nb()
                    for h in range(8):
                        S.matmul(blk(ps3, h), mmd(blk(Pt2, h)), mmd(blk(Rm, h)))
                    S.tt("dve", Rm[:], Rm[:], ps3[0:64, :], ALU.add)
                    if lev == 4:
                        S.copy("pool", Rmb[:], Rm[:])
                    P, Pt, P2, Pt2 = P2, Pt2, P, Pt
                    yield
                psW = nb()
                for pr in range(4):
                    S.matmul(pp(psW, pr), pl(kkt, pr), Hbb[:, pr, :], start=True, stop=False)
                    for hp in range(2):
                        h = 2 * pr + hp
                        S.matmul(blk(psW, h), blk(AkkT, h), vtok[:, ch, h * 64:(h + 1) * 64], start=False, stop=(hp == 1))
                S.copy("act", Wsb[:], psW[0:64, :])
                yield
                psU = nb()
                for h in range(8):
                    S.matmul(blk(psU, h), blk(Rmb, h), blk(Wsb, h))
                S.ts("dve", Un[:], psU[0:64, :], -1.0, ALU.mult)
                yield
                psY = nb()
                psH = nb()
                for pr in range(4):
                    S.matmul(psY[:, pr * 64:(pr + 1) * 64], Hbb[:, pr, :], pl(rt, pr), start=True, stop=False)
                    for hp in range(2):
                        h = 2 * pr + hp
                        oy = psY[hp * 64:(hp + 1) * 64, pr * 64:(pr + 1) * 64]
                        S.matmul(oy, vtok[:, ch, h * 64:(h + 1) * 64], blk(ArkT, h), start=False, stop=False)
                        S.matmul(oy, blk(Un, h), blk(ArbT, h), start=False, stop=True)
                for pr in range(4):
                    for hp in range(2):
                        h = 2 * pr + hp
                        oh = psH[hp * 64:(hp + 1) * 64, h * 64:(h + 1) * 64]
                        S.matmul(oh, kbtok[:, ch, h * 64:(h + 1) * 64], vtok[:, ch, h * 64:(h + 1) * 64], start=True, stop=False)
                        S.matmul(oh, bbtok[:, ch, h * 64:(h + 1) * 64], blk(Un, h), start=False, stop=True)
                S.copy("act", yt.v(yt.t[:, :, c0:c0 + LR]), psY.v(psY.t[:, 0:256].rearrange("p (c t) -> p c t", t=64)))
                for pr in range(4):
                    for hp in range(2):
                        h = 2 * pr + hp
                        rows = slice(hp * 64, hp * 64 + 64)
                        S.stt(Hbd[rows, pr, hp * 64:hp * 64 + 64], Hbd[rows, pr, hp * 64:hp * 64 + 64], gg[rows, pr, c0 + ie:c0 + ie + 1],
                              psH[rows, h * 64:(h + 1) * 64], ALU.mult, ALU.add, e="dve")
                S.copy("pool", Hbb[:], Hbd[:])
                yield
            S.dma("pool", fm((st.Y0, st.Y1)[d], 0, 512, *sl), yt[:, :, 0:n])


TSS = 128
INV_FP32R = False


def interleave(gens):
    gens = list(gens)
    while gens:
        for g in list(gens):
            try:
                next(g)
            except StopIteration:
                gens.remove(g)


def scan0_both(G):
    G.S.push()
    interleave([scan0_gen(G, 0), scan0_gen(G, 1)])
    G.S.pop()


def out_phase(G, l):
    S, nc, fm, vc, cc, nb, ew = G.S, G.nc, G.fm, G.vc, G.cc, G.nb, G.ew
    W = G.w
    S.push()
    wo = S.sbuf("o_wo", [128, KD, D], BF16)
    wu = S.sbuf("o_wu", [128, KD, 2 * DFF], BF16)
    stage = [S.sbuf(f"o_st{i}", [128, KD, 256], F32) for i in range(2)]
    wsrc = W["ab_w_out"] if l == 0 else W["c_w_out"]
    G.load_w_bf16(wo, lambda c0, c1: wsrc.v(wsrc.t[:, c0:c1].rearrange("(j p) f -> p j f", p=128)), D, stage)
    fu = W["ffn_up"]
    G.load_w_bf16(wu, lambda c0, c1: fu.v(fu.t[l, :, c0:c1].rearrange("(j p) f -> p j f", p=128)), 2 * DFF, stage)
    f8 = lambda nm: S.sbuf(f"o_{nm}", [128, KD, TS], F32)
    xin = f8("xin")
    mo = S.sbuf("o_mo", [128, KD, TS], BF16)
    hf = S.sbuf("o_hf", [128, KD, TS], BF16)
    rstd = S.sbuf("o_rstd", [128, TS], F32)
    tmp = S.sbuf("o_tmp", [128, TS], F32)
    gcm = S.sbuf("o_gcm", [128, 8], F32)
    cff = S.sbuf("o_cff", [128, 8], F32)
    fab = [S.sbuf(f"o_fab{i}", [128, 2, TS], F32) for i in range(2)]
    faa = [S.sbuf(f"o_faa{i}", [128, 2, TS], BF16) for i in range(2)]
    a1, a2, a3, a4 = f8("a1"), f8("a2"), f8("a3"), f8("a4")
    ymix, sq, x1 = a2, a3, a4
    streams = (G.ctx, G.lat) if l == 0 else (G.lat,)
    for st in streams:
        T = st.T
        G.make_gcoef(l, st, f"nmo{l}", 2, gcm[:])
        G.make_coef(l, st, f"nfp{l}", 4, cff[:])
        for (t0, n) in G.tiles(T):
            sl = (t0, t0 + n)
            S.dma("sp", xin[:, :, 0:n], fm(st.res[2 * l], 0, D, *sl))
            if l == 0:
                S.dma("sp", mo[:, 0:4, 0:n], fm(st.AO, 0, 512, *sl))
                y0, y1, gt, bvt = a1, a2, a3, a4
                S.dma("sp", y0[:, 0:4, 0:n], fm(st.Y0, 0, 512, *sl))
                S.dma("sp", y1[:, 0:4, 0:n], fm(st.Y1, 0, 512, *sl))
                S.dma("sp", gt[:, 0:4, 0:n], fm(st.GG, 0, 512, *sl))
                S.dma("sp", bvt[:, 0:4, 0:n], fm(st.BV, 0, 512, *sl))
                S.tt("dve", y0[:, 0:4, 0:n], y0[:, 0:4, 0:n], y1[:, 0:4, 0:n], ALU.add)
                for c in range(4):
                    ps = nb()
                    S.matmul(ps[:, 0:n], cc("blk64"), y0[:, c, 0:n])
                    S.stt(y0[:, c, 0:n], ps[:, 0:n], -1.0 / 64, y0[:, c, 0:n], ALU.mult, ALU.add)
                    S.act(y1[:, c, 0:n], y0[:, c, 0:n], AF.Square)
                    G.rstd_bcast([y1[:, c, 0:n]], n, rstd[:, 0:n], cc("blk64"), 1.0 / 64, LN_EPS, tmp[:, 0:n])
                    S.tt("dve", y0[:, c, 0:n], y0[:, c, 0:n], rstd[:, 0:n], ALU.mult)
                    S.ts("dve", y0[:, c, 0:n], y0[:, c, 0:n], vc("blng", c), ALU.mult, vc("blnb", c), ALU.add)
                S.tt("dve", y0[:, 0:4, 0:n], y0[:, 0:4, 0:n], bvt[:, 0:4, 0:n], ALU.add)
                S.tt("dve", mo[:, 4:8, 0:n], y0[:, 0:4, 0:n], gt[:, 0:4, 0:n], ALU.mult)
            else:
                h0, h1, xcv, sz = a1, a2, a3, a4
                S.dma("sp", h0[:, :, 0:n], fm(st.H0, 0, D, *sl))
                S.dma("sp", h1[:, :, 0:n], fm(st.H1, 0, D, *sl))
                S.dma("sp", xcv[:, :, 0:n], fm(st.XCV, 0, D, *sl))
                S.dma("sp", sz[:, :, 0:n], fm(st.SZ, 0, D, *sl))
                S.tt("dve", h0[:, :, 0:n], h0[:, :, 0:n], h1[:, :, 0:n], ALU.add)
                S.act(h1[:, :, 0:n], h0[:, :, 0:n], AF.Square)
                for hd in range(4):
                    G.rstd_bcast([h1[:, 2 * hd, 0:n], h1[:, 2 * hd + 1, 0:n]], n, rstd[:, 0:n], cc("ones"), 1.0 / 256, EPS, tmp[:, 0:n])
                    for j in (2 * hd, 2 * hd + 1):
                        S.stt(h0[:, j, 0:n], h0[:, j, 0:n], vc("cnorm", j), rstd[:, 0:n], ALU.mult, ALU.mult)
                        S.stt(h0[:, j, 0:n], xcv[:, j, 0:n], vc("cskip", j), h0[:, j, 0:n], ALU.mult, ALU.add)
                S.tt("dve", mo[:, :, 0:n], h0[:, :, 0:n], sz[:, :, 0:n], ALU.mult)
            for oc in range(KD):
                ps = nb()
                for k in range(KD):
                    S.matmul(ps[:, 0:n], wo[:, k, oc * 128:(oc + 1) * 128], mo[:, k, 0:n], start=(k == 0), stop=(k == KD - 1))
                S.copy("act", ymix[:, oc, 0:n], ps[:, 0:n])
            G.post_norm_res(ymix, n, gcm, xin, x1, sq, rstd, tmp, sqb=hf)
            S.dma("pool", fm(st.res[2 * l + 1], 0, D, *sl), x1[:, :, 0:n])
            G.norm_mod(x1, n, cff, 3, st, hf, sq, rstd, tmp)
            for og in range(NF):
                fb = (faa if og < NF // 2 else fab)[og % 2]
                for o2 in range(2):
                    oc = og * 2 + o2
                    ps = nb()
                    for k in range(KD):
                        S.matmul(ps[:, 0:n], wu[:, k, oc * 128:(oc + 1) * 128], hf[:, k, 0:n], start=(k == 0), stop=(k == KD - 1))
                    S.copy("act" if oc % 2 else "dve", fb[:, o2, 0:n], ps[:, 0:n])
                dstb = st.FA if og < NF // 2 else st.FB
                r0 = (og * 2 % NF) * 128
                S.dma("pool", fm(dstb, r0, r0 + 256, *sl), fb[:, :, 0:n])
    S.pop()


def ffn2_phase(G, l):
    S, nc, fm, vc, cc, nb, ew = G.S, G.nc, G.fm, G.vc, G.cc, G.nb, G.ew
    W = G.w
    S.push()
    GW = 64
    wd = S.sbuf("f_wd", [128, NF, D], BF16)
    at = S.sbuf("f_at", [128, NF, TS + 2 * GW + 2], BF16)
    S.memset("pool", at[:], 0.0)
    GP = GW + 1
    nfc = S.sbuf("f_nfc", [128, 9 * NF], F32)
    S.ts("dve", nfc[:], vc(f"fconv{l}_0", 0, 9 * NF), -1.0, ALU.mult)
    bt = S.sbuf("f_bt", [128, NF, TS], F32)
    xin0 = S.sbuf("f_xin0", [128, KD, TS], F32)
    stage = [bt[:, :, 0:256], xin0.v(xin0.t[:].rearrange("p a b -> p (a b)")[:, 0:NF * 64].rearrange("p (a b) -> p a b", b=64))]
    dg = S.sbuf("f_dg", [128, NF * 9, 128], BF16)
    for j in range(NF):
        for ti in range(9):
            if ti % 2:
                S.act(dg[:, j * 9 + ti, :], cc("ident"), AF.Copy, scale=vc(f"fconv{l}_{ti}", j))
            else:
                S.ts("dve", dg[:, j * 9 + ti, :], cc("ident"), vc(f"fconv{l}_{ti}", j), ALU.mult)
    fd = W["ffn_down"]
    c0 = 0
    i = 0
    while c0 < D:
        stg = stage[0]
        S.dma("sp", stg, fd.v(fd.t[l, :, c0:c0 + 256].rearrange("(j p) f -> p j f", p=128)))
        S.copy("dve" if i % 2 else "act", wd[:, :, c0:c0 + 256], stg)
        c0 += 256
        i += 1
    cv_2 = [S.sbuf("f_cv%d" % i, [128, TS], F32) for i in range(3)]
    gb = S.sbuf("f_gb", [128, NF, TS], BF16)
    f8 = lambda nm: S.sbuf(f"f_{nm}", [128, KD, TS], F32)
    xin, yf, sq = xin0, f8("yf"), f8("sq")
    xo = yf
    rstd = S.sbuf("f_rstd", [128, TS], F32)
    tmp = S.sbuf("f_tmp", [128, TS], F32)
    gcf = S.sbuf("f_gcf", [128, 8], F32)
    streams = (G.ctx, G.lat) if l == 0 else (G.lat,)
    for st in streams:
        T = st.T
        G.make_gcoef(l, st, f"nfo{l}", 5, gcf[:])
        isl = st is G.lat
        for (t0, n) in G.tiles(T):
            sl = (t0, t0 + n)
            if isl:
                Wd, R = GW, n // GW
                lo, hi = t0 - GW, t0 + n + GW
                clo, chi = max(lo, 0), min(hi, T)
                if lo < 0:
                    S.memset("pool", at[:, :, 1:1 + GW], 0.0)
                if hi > T:
                    S.memset("pool", at[:, :, GP + n:GP + n + GW], 0.0)
                S.dma("sp", at[:, :, 1 + clo - lo:1 + chi - lo], fm(st.FA, 0, DFF, clo, chi))
                taps = [(dr, dc) for dr in (-1, 0, 1) for dc in (-1, 0, 1)]
            else:
                assert T <= TS
                Wd, R = n, 1
                S.memset("pool", at[:, :, GP - 1:GP], 0.0)
                S.memset("pool", at[:, :, GP + n:GP + n + 1], 0.0)
                S.dma("sp", at[:, :, GP:GP + n], fm(st.FA, 0, DFF, t0, t0 + n))
                taps = [(0, -1), (0, 0), (0, 1)]
            S.dma("sp", bt[:, :, 0:n], fm(st.FB, 0, DFF, *sl))
            S.dma("sp", xin[:, :, 0:n], fm(st.res[2 * l + 1], 0, D, *sl))
            for j in range(NF):
                cv = cv_2[j % 3]

                pc = nb()
                order = [(0, 0)] + [t_ for t_ in taps if t_ != (0, 0)]
                for ti_, (dr, dc) in enumerate(order):
                    w_ = dg[:, j * 9 + (dr + 1) * 3 + (dc + 1), :]
                    b0 = GP + dr * Wd + dc
                    S.matmul(pc[:, 0:n], w_, at[:, j, b0:b0 + n], start=(ti_ == 0), stop=(ti_ == len(order) - 1))
                if isl:
                    for dr in (-1, 0, 1):
                        for dc in (-1, 1):
                            col = 0 if dc == -1 else Wd - 1
                            a0 = GP + dr * Wd + dc + col
                            a_ = at.v(at.t[:, j, a0:a0 + (R - 1) * Wd + 1:Wd], j)
                            o_ = pc.v(pc.t[:, col:col + (R - 1) * Wd + 1:Wd])
                            ti = (dr + 1) * 3 + (dc + 1)
                            S.stt(o_, a_, nfc[:, ti * NF + j:ti * NF + j + 1], o_, ALU.mult, ALU.add)
                S.act(cv[:, 0:n], pc[:, 0:n], AF.Gelu_apprx_tanh, bias=vc(f"fcb{l}", j), scale=1.0)
                S.tt("dve" if j % 2 else "pool", gb[:, j, 0:n], cv[:, 0:n], bt[:, j, 0:n], ALU.mult)
            for oc in range(KD):
                ps = nb()
                for k in range(NF):
                    S.matmul(ps[:, 0:n], wd[:, k, oc * 128:(oc + 1) * 128], gb[:, k, 0:n], start=(k == 0), stop=(k == NF - 1))
                S.copy("act", yf[:, oc, 0:n], ps[:, 0:n])
            G.post_norm_res(yf, n, gcf, xin, xo, sq, rstd, tmp, sqb=gb)
            S.dma("pool", fm(st.res[2 * l + 2], 0, D, *sl), xo[:, :, 0:n])
    S.pop()


def in1_phase(G):
    S, nc, fm, vc, cc, nb, ew = G.S, G.nc, G.fm, G.vc, G.cc, G.nb, G.ew
    W = G.w
    S.push()
    NH = TS + 2
    wb = S.sbuf("i1_wb", [128, KD, 2064], BF16)
    stage = [S.sbuf(f"i1_st{i}", [128, KD, 512], F32) for i in range(2)]
    G.load_w_bf16(wb, lambda c0, c1: W["c_w_in"].v(W["c_w_in"].t[:, c0:c1].rearrange("(j p) f -> p j f", p=128)), 2064, stage)
    bdf = S.sbuf("i1_bdf", [128, 3, KD, 128], F32)
    S.dma("sp", bdf[:], W["c_bd"][:])
    coef = S.sbuf("i1_coef", [128, 8], F32)
    xt = S.sbuf("i1_xt", [128, KD, NH], F32)
    sq = S.sbuf("i1_sq", [128, KD, NH], F32)
    rstd = S.sbuf("i1_rstd", [128, NH], F32)
    tmp = S.sbuf("i1_tmp", [128, NH], F32)
    h = S.sbuf("i1_h", [128, KD, NH], BF16)
    xm = S.sbuf("i1_xm", [128, KD, NH], F32)
    xcv = S.sbuf("i1_xcv", [128, KD, TS], F32)
    szt = S.sbuf("i1_sz", [128, KD, TS], F32)
    qt = S.sbuf("i1_q", [128, KD, TS], F32)
    kt = S.sbuf("i1_k", [128, KD, TS], F32)
    ktok = S.sbuf("i1_ktok", [128, TS // 128, D], F32)
    vtok = S.sbuf("i1_vtok", [128, TS // 128, D], F32)
    gat = S.sbuf("i1_gat", [16, TS], F32)
    ct = S.sbuf("i1_ct", [128, TS], F32)
    for st in (G.ctx, G.lat):
        T = st.T
        G.make_coef(1, st, "nmp1", 1, coef[:])
        for (t0, n) in G.tiles(T):
            nh = n + 2
            sl = (t0, t0 + n)
            G.load_halo(xt, st.res[2], t0, n, T, 0, D)
            G.norm_mod(xt, nh, coef, 0, st, h, sq, rstd, tmp)

            def proj(oc, msz=128):
                ps = nb()
                for k in range(KD):
                    S.matmul(ps[0:msz, 0:nh], wb[:, k, oc * 128:oc * 128 + msz], h[:, k, 0:nh], start=(k == 0), stop=(k == KD - 1))
                return ps
            for j in range(KD):
                ps = proj(j)
                S.copy("act", xm[:, j, 0:nh], ps[:, 0:nh])
            if t0 == 0:
                S.memset("pool", xm[:, :, 0:1], 0.0)
            if t0 + n >= T:
                S.memset("pool", xm[:, :, n + 1:n + 2], 0.0)
            for j in range(KD):
                ps = proj(KD + j)
                S.act(szt[:, j, 0:n], ps[:, 1:n + 1], AF.Sigmoid)
            ps = proj(2 * KD, 16)
            S.act(gat[0:16, 0:n], ps[0:16, 1:n + 1], AF.Identity, bias=G.vcr("cgb", 16), scale=1.0)
            S.dma("pool", st.GATE[:, t0:t0 + n], gat[:, 0:n])
            for j in range(KD):
                S.ts("dve", ct[:, 0:n], xm[:, j, 1:n + 1], vc("cconv1", j), ALU.mult, vc("ccb", j), ALU.add)
                S.stt(ct[:, 0:n], xm[:, j, 0:n], vc("cconv0", j), ct[:, 0:n], ALU.mult, ALU.add)
                S.stt(ct[:, 0:n], xm[:, j, 2:n + 2], vc("cconv2", j), ct[:, 0:n], ALU.mult, ALU.add)
                S.act(xcv[:, j, 0:n], ct[:, 0:n], AF.Silu)
            for j in range(KD):
                ps = nb()
                S.matmul(ps[:, 0:n], bdf[:, 0, j, :], xcv[:, j, 0:n])
                S.copy("act", qt[:, j, 0:n], ps[:, 0:n])
                ps = nb()
                S.matmul(ps[:, 0:n], bdf[:, 1, j, :], xcv[:, j, 0:n])
                S.ts("dve", kt[:, j, 0:n], ps[:, 0:n], 1.0 / 16, ALU.mult)
                for cch in range(n // 128):
                    ps = nb()
                    S.matmul(ps[:, 0:128], xcv[:, j, cch * 128:(cch + 1) * 128], bdf[:, 1, j, :])
                    S.ts("dve", ktok[:, cch, j * 128:(j + 1) * 128], ps[:, 0:128], 1.0 / 16, ALU.mult)
                    ps = nb()
                    S.matmul(ps[:, 0:128], xm[:, j, 1 + cch * 128:1 + (cch + 1) * 128], bdf[:, 2, j, :])
                    S.copy("act", vtok[:, cch, j * 128:(j + 1) * 128], ps[:, 0:128])
            S.dma("pool", fm(st.MQ, 0, D, *sl), qt[:, :, 0:n])
            S.dma("pool", fm(st.MK, 0, D, *sl), kt[:, :, 0:n])
            S.dma("pool", fm(st.XCV, 0, D, *sl), xcv[:, :, 0:n])
            S.dma("pool", fm(st.SZ, 0, D, *sl), szt[:, :, 0:n])
            for cch in range(n // 128):
                S.dma("pool", st.KTOK[t0 + cch * 128:t0 + (cch + 1) * 128, :], ktok[:, cch, :])
                S.dma("pool", st.VTOK[t0 + cch * 128:t0 + (cch + 1) * 128, :], vtok[:, cch, :])
    S.pop()


def scan1_gen(G, d, h0):
    S, nc, fm, vc, cc, nb, ew = G.S, G.nc, G.fm, G.vc, G.cc, G.nb, G.ew
    L = LM
    NHD = 2
    qtf = S.sbuf("s1_qf", [128, 2 * NHD, L], F32)
    ktf = S.sbuf("s1_kf", [128, 2 * NHD, L], F32)
    ktok = S.sbuf("s1_ktok", [128, NHD * 256], F32)
    vtokf = S.sbuf("s1_vtokf", [128, NHD * 256], F32)
    qt = S.sbuf("s1_q", [128, 2 * NHD, L], BF16)
    kt = S.sbuf("s1_k", [128, 2 * NHD, L], BF16)
    vtok = S.sbuf("s1_vtok", [128, NHD * 256], BF16)
    C0b = S.sbuf("s1_C0b", [128, NHD, 2, 256], BF16)
    n0b = S.sbuf("s1_n0b", [128, NHD, 2, 128], BF16)
    onesb = S.sbuf("s1_onesb", [128, 128], BF16)
    S.memset("dve", C0b[:], 0.0)
    S.memset("dve", n0b[:], 0.0)
    S.memset("dve", onesb[:], 1.0)
    g16 = S.sbuf("s1_g16", [16, L], F32)
    lf = S.sbuf("s1_lf", [16, L], F32)
    b16 = S.sbuf("s1_b16", [16, L], F32)
    t16 = S.sbuf("s1_t16", [16, L], F32)
    T1 = S.sbuf("s1_T1", [128, 16], F32)
    T2 = S.sbuf("s1_T2", [128, 16], F32)
    CT = S.sbuf("s1_CT", [128, 4], F32)
    Bbs = S.sbuf("s1_Bbs", [128, L], F32)
    EQb = S.sbuf("s1_EQb", [128, L], F32)
    Dm = S.sbuf("s1_Dm", [128, L], F32)
    STs = S.sbuf("s1_STs", [128, L], BF16)
    ekb = S.sbuf("s1_ekb", [128, 1], F32)
    Kbar = S.sbuf("s1_Kbar", [128, 256], BF16)
    C0 = S.sbuf("s1_C0", [128, NHD, 2, 256], F32)
    n0 = S.sbuf("s1_n0", [128, NHD, 2, 128], F32)
    den = S.sbuf("s1_den", [128, L], F32)
    num = S.sbuf("s1_num", [128, L], F32)
    ht = S.sbuf("s1_h", [128, 2 * NHD, L], F32)
    S.memset("dve", C0[:], 0.0)
    S.memset("pool", n0[:], 0.0)
    mask = cc("mu") if d == 0 else cc("ml")
    iL = (L - 1) if d == 0 else 0
    for st in (G.ctx, G.lat):
        T = st.T
        chunks = list(range(0, T, L))
        if d == 1:
            chunks = chunks[::-1]
        for t0 in chunks:
            sl = (t0, t0 + L)
            r0, r1 = h0 * 256, (h0 + NHD) * 256
            S.dma("sp", qtf[:], fm(st.MQ, r0, r1, *sl))
            S.dma("sp", ktf[:], fm(st.MK, r0, r1, *sl))
            S.dma("sp", ktok[:], st.KTOK[t0:t0 + L, r0:r1])
            S.dma("sp", vtokf[:], st.VTOK[t0:t0 + L, r0:r1])
            S.copy("dve", qt[:], qtf[:])
            S.copy("pool", kt[:], ktf[:])
            S.copy("act", vtok[:], vtokf[:])
            S.dma("sp", g16[:], st.GATE[:, t0:t0 + L])
            S.act(lf[:], g16[:], AF.Sigmoid)
            S.act(lf[:], lf[:], AF.Ln)
            S.op("dve", lambda: nc.vector.tensor_tensor_scan(b16.t[:, :], cc("ones", 16, 0, L).ap, lf.t[:, :], 0.0, ALU.mult, ALU.add),
                 reads=[cc("ones"), lf[:]], writes=[b16[:]])
            if d == 1:
                S.tt("dve", t16[:], b16.v(b16.t[:, L - 1:L].broadcast_to([16, L])), b16[:], ALU.subtract)
                S.tt("dve", b16[:], t16[:], lf[:], ALU.add)
            p1 = nb()
            S.transpose(p1[:, 0:16], g16[:], cc("ident", 16, 0, 16))
            S.copy("act", T1[:], p1[:, 0:16])
            p2 = nb()
            S.transpose(p2[:, 0:16], b16[:], cc("ident", 16, 0, 16))
            S.copy("act", T2[:], p2[:, 0:16])
            S.tt("dve", CT[:], T1[:, 4 * d:4 * d + 4], T2[:, 8 + 4 * d:8 + 4 * d + 4], ALU.subtract)
            for h in range(NHD):
                r = 8 + 4 * d + h0 + h
                hg = h0 + h
                psB = nb()
                S.matmul(psB[:, 0:L], cc("sel", 16, r * 128, 128), b16[:])
                S.copy("act", Bbs[:], psB[:, 0:L])
                S.act(EQb[:], psB[:, 0:L], AF.Exp)
                S.act(Dm[:], Bbs[:], AF.Exp, bias=CT[:, hg:hg + 1], scale=1.0)
                S.tt("pool", Dm[:], Dm[:], mask, ALU.mult)
                psS = nb()
                for kc in range(2):
                    S.matmul(psS[:, 0:L], kt[:, 2 * h + kc, :], qt[:, 2 * h + kc, :], start=(kc == 0), stop=(kc == 1))
                S.tt("dve", STs[:], psS[:, 0:L], Dm[:], ALU.mult)
                psD = nb()
                S.matmul(psD[:, 0:L], onesb[:], STs[:])
                psE = nb()
                for kc in range(2):
                    S.matmul(psE[:, 0:L], n0b[:, h, kc, :], qt[:, 2 * h + kc, :], start=(kc == 0), stop=(kc == 1))
                S.tt("dve", den[:], psE[:, 0:L], EQb[:], ALU.mult)
                S.tt("dve", den[:], den[:], psD[:, 0:L], ALU.add)
                S.act(den[:], den[:], AF.Abs)
                S.ts("dve", den[:], den[:], 1.0, ALU.max)
                S.op("dve", lambda: nc.vector.reciprocal(den.t[:, :], den.t[:, :]), reads=[den[:]], writes=[den[:]])
                yield
                for vcx in range(2):
                    psI = nb()
                    S.matmul(psI[:, 0:L], vtok[:, h * 256 + vcx * 128:h * 256 + (vcx + 1) * 128], STs[:])
                    psX = nb()
                    for kc in range(2):
                        S.matmul(psX[:, 0:L], C0b[:, h, kc, vcx * 128:(vcx + 1) * 128], qt[:, 2 * h + kc, :], start=(kc == 0), stop=(kc == 1))
                    S.tt("dve", num[:], psX[:, 0:L], EQb[:], ALU.mult)
                    S.tt("dve", num[:], num[:], psI[:, 0:L], ALU.add)
                    S.tt("pool", ht[:, 2 * h + vcx, :], num[:], den[:], ALU.mult)
                S.act(ekb[:], CT[:, hg:hg + 1], AF.Exp, bias=Bbs[:, iL:iL + 1], scale=1.0)
                S.ts("dve", Kbar[:], ktok[:, h * 256:(h + 1) * 256], ekb[:, 0:1], ALU.mult)
                for kc in range(2):
                    psC = nb()
                    S.matmul(psC[:, 0:256], Kbar[:, kc * 128:(kc + 1) * 128], vtok[:, h * 256:(h + 1) * 256])
                    S.stt(C0[:, h, kc, :], C0[:, h, kc, :], EQb[:, iL:iL + 1], psC[:, 0:256], ALU.mult, ALU.add)
                    S.copy("act", C0b[:, h, kc, :], C0[:, h, kc, :])
                    psN = nb()
                    S.matmul(psN[:, 0:128], Kbar[:, kc * 128:(kc + 1) * 128], onesb[:])
                    S.stt(n0[:, h, kc, :], n0[:, h, kc, :], EQb[:, iL:iL + 1], psN[:, 0:128], ALU.mult, ALU.add)
                    S.copy("act", n0b[:, h, kc, :], n0[:, h, kc, :])
                yield
            if st is G.lat:
                S.dma("pool", fm((st.H0, st.H1)[d], r0, r1, *sl), ht[:])


def scan1_both(G):

    G.S.push()
    interleave([scan1_gen(G, 0, 0), scan1_gen(G, 1, 0), scan1_gen(G, 0, 2), scan1_gen(G, 1, 2)])
    G.S.pop()


def build_all(TL, TC, upto=99):
    G = build(TL, TC)
    S = G.S
    steps = [
        lambda: G.ada(0), lambda: in0_phase(G), lambda: scan0_both(G),
        lambda: out_phase(G, 0), lambda: ffn2_phase(G, 0),
        lambda: G.ada(1), lambda: in1_phase(G), lambda: scan1_both(G),
        lambda: out_phase(G, 1), lambda: ffn2_phase(G, 1),
    ]
    for i, f in enumerate(steps):
        if i >= upto:
            break
        f()
    S.wait_all("pool")
    S.wait_all("sp")
    S.es.close()
    return G


def colvec(v):
    v = np.asarray(v, np.float32).reshape(-1)
    assert v.size % 128 == 0
    return v.reshape(-1, 128).T


def pack_shared(inp):
    vlay, NV = vec_layout()
    vecs = np.zeros((128, NV), np.float32)

    def put(name, v):
        o, n = vlay[name]
        cv = colvec(v)
        assert cv.shape[1] == n, (name, cv.shape, n)
        vecs[:, o:o + n] = cv
    for l in range(2):
        put(f"nmp{l}", inp["norm_mix_pre"][l])
        put(f"nmo{l}", inp["norm_mix_post"][l])
        put(f"nfp{l}", inp["norm_ffn_pre"][l])
        put(f"nfo{l}", inp["norm_ffn_post"][l])
        put(f"adab{l}", inp["ada_b"][l])
        for i in range(9):
            put(f"fconv{l}_{i}", inp["ffn_conv"][l].reshape(9, DFF)[i])
        put(f"fcb{l}", inp["ffn_conv_b"][l])
    put("agv", inp["a_gv"][0])
    for i in range(3):
        put(f"bshift{i}", inp["b_shift"][0][i])
    put("bw0", inp["b_w0"][0])
    put("ba0", inp["b_a0"][0])
    put("bkk", inp["b_kk"][0])
    put("bka", inp["b_ka"][0])
    put("brk", inp["b_rk"][0])
    put("blng", inp["b_ln_g"][0])
    put("blnb", inp["b_ln_b"][0])
    for i in range(3):
        put(f"cconv{i}", inp["c_conv"][0][i])
    put("ccb", inp["c_conv_b"][0])
    put("cskip", inp["c_skip"][0])
    put("cnorm", inp["c_norm"][0])
    o, _ = vlay["cgb"]
    vecs[0:8, o] = np.asarray(inp["c_bi"][0]).reshape(-1)
    vecs[8:16, o] = np.asarray(inp["c_bf"][0]).reshape(-1)
    sh = {"vecs": vecs, "consts": make_consts()}
    f32 = lambda a: np.ascontiguousarray(np.asarray(a, np.float32))
    sh["ada_w"] = f32(inp["ada_w"])
    sh["ffn_up"] = f32(inp["ffn_up"])
    sh["ffn_down"] = f32(inp["ffn_down"])
    sh["ab_w_in"] = f32(inp["ab_w_in"][0])
    sh["ab_w_out"] = f32(inp["ab_w_out"][0])
    sh["c_w_in"] = f32(inp["c_w_in"][0])
    sh["c_w_out"] = f32(inp["c_w_out"][0])
    sh["a_wsT"] = f32(np.transpose(np.asarray(inp["a_ws"][0]), (2, 0, 1)))
    sh["a_bs_bc"] = f32(np.broadcast_to(np.asarray(inp["a_bs"][0]).reshape(1, 512), (128, 512)))
    bup = np.zeros((128, 5, 512), np.float32)
    wu_, au_ = np.asarray(inp["b_w_up"][0]), np.asarray(inp["b_a_up"][0])
    for d_ in range(2):
        bup[d_ * 64:(d_ + 1) * 64, d_, :] = wu_[d_]
        bup[d_ * 64:(d_ + 1) * 64, 2 + d_, :] = au_[d_]
    bup[:, 4, :] = np.asarray(inp["b_g_up"][0])
    sh["b_up"] = bup
    bd = np.zeros((128, 3, KD, 128), np.float32)
    for wi, nm in enumerate(("c_wq", "c_wk", "c_wv")):
        w = np.asarray(inp[nm][0])
        for g in range(256):
            j, gl = divmod(g, 32)
            bd[gl * 4:gl * 4 + 4, wi, j, gl * 4:gl * 4 + 4] = w[g]
    sh["c_bd"] = bd
    return sh


def pack_core(inp, b, sh):
    m = dict(sh)
    m["xT"] = np.ascontiguousarray(np.asarray(inp["x"][b], np.float32).T)
    m["cxT"] = np.ascontiguousarray(np.asarray(inp["ctx"][b], np.float32).T)
    cv = np.stack([np.asarray(inp["c"][b], np.float32), np.asarray(inp["c_ctx"], np.float32)], axis=-1)
    m["cvec"] = np.ascontiguousarray(cv.reshape(KD, 128, 2).transpose(1, 0, 2))
    return m


TL_FULL = 8192
TC_FULL = 256
_CACHE = {}


def kernel(**inputs):
    inp = {k: np.asarray(v) for k, v in inputs.items()}
    B = inp["x"].shape[0]
    if "G" not in _CACHE:
        _CACHE["G"] = build_all(TL_FULL, TC_FULL)
    G = _CACHE["G"]
    sh = pack_shared(inp)
    ncores = 8
    in_maps = [pack_core(inp, c % B, sh) for c in range(ncores)]
    res = run_bass_kernel_spmd(G.nc, in_maps, core_ids=list(range(ncores)))
    out = np.stack([np.ascontiguousarray(res.results[b]["outT"].T) for b in range(B)], axis=0)
    return out.astype(np.float32)
```
